# Optimizing a Trainium2 kernel written in Bass

```python
import math
import jax, jax.numpy as jnp
from jax import lax
import numpy as np

D_MODEL = 1024
BATCH = 8
SEQ = 4096
DEPTH = 2

N_A_LAYERS = DEPTH // 2
N_B_LAYERS = DEPTH - N_A_LAYERS
N_DENSE = (DEPTH + 1) // 2
N_MOE = DEPTH // 2
MLA_HEADS = 8
QK_NOPE = 128
QK_ROPE = 64
V_DIM = 128
Q_LORA = 384
KV_LORA = 256
ROPE_THETA = 10000.0
SB_HEADS = 8
SB_HEAD_DIM = 128
D_FF = 3584
N_EXPERTS = 8
TOP_K = 2
Q_BLOCK = 128
EPS = 1e-6
COND_MULT = 6

kernel_name = "yoco_mla_stickbreaking_moe_adaln"


def rms_norm(x, g):
    xf = x.astype(jnp.float32)
    y = xf * lax.rsqrt(jnp.mean(xf * xf, axis=-1, keepdims=True) + EPS)
    return (y * g.astype(jnp.float32)).astype(x.dtype)


def modulate(h, shift, scale):
    return h * (1 + scale[:, None, :]) + shift[:, None, :]


def rope(x, pos):
    half = x.shape[-1] // 2
    inv = ROPE_THETA ** (-jnp.arange(half, dtype=jnp.float32) / half)
    ang = pos.astype(jnp.float32)[..., None] * inv
    cos = jnp.cos(ang)[:, :, None, :]
    sin = jnp.sin(ang)[:, :, None, :]
    xf = x.astype(jnp.float32)
    x1, x2 = xf[..., :half], xf[..., half:]
    return jnp.concatenate([x1 * cos - x2 * sin, x1 * sin + x2 * cos], axis=-1).astype(x.dtype)


def _split_query_blocks(q):
    b, s, h, d = q.shape
    nb = s // Q_BLOCK
    return q.reshape(b, nb, Q_BLOCK, h, d).swapaxes(0, 1), nb


def _merge_query_blocks(o):
    nb, b, qb, h, d = o.shape
    return o.swapaxes(0, 1).reshape(b, nb * qb, h, d)


def causal_softmax_attention(q, k, v):
    scale = 1.0 / math.sqrt(q.shape[-1])
    qb_all, nb = _split_query_blocks(q)
    kpos = jnp.arange(k.shape[1])

    def block(args):
        qb, bi = args
        s = jnp.einsum('bqhd,bkhd->bhqk', qb, k).astype(jnp.float32) * scale
        qpos = bi * Q_BLOCK + jnp.arange(Q_BLOCK)
        mask = kpos[None, :] <= qpos[:, None]
        p = jax.nn.softmax(jnp.where(mask, s, -jnp.inf), axis=-1)
        return jnp.einsum('bhqk,bkhd->bqhd', p.astype(v.dtype), v)

    return _merge_query_blocks(lax.map(block, (qb_all, jnp.arange(nb))))


def stick_breaking_attention(q, k, v):
    scale = 1.0 / math.sqrt(q.shape[-1])
    qb_all, nb = _split_query_blocks(q)
    kpos = jnp.arange(k.shape[1])

    def block(args):
        qb, bi = args
        z = jnp.einsum('bqhd,bkhd->bhqk', qb, k).astype(jnp.float32) * scale
        qpos = bi * Q_BLOCK + jnp.arange(Q_BLOCK)
        mask = kpos[None, :] < qpos[:, None]
        log_beta = jax.nn.log_sigmoid(z)
        log_1m = jnp.where(mask, log_beta - z, 0.0)
        suffix = lax.cumsum(log_1m, axis=3, reverse=True) - log_1m
        a = jnp.where(mask, jnp.exp(log_beta + suffix), 0.0)
        return jnp.einsum('bhqk,bkhd->bqhd', a.astype(v.dtype), v)

    return _merge_query_blocks(lax.map(block, (qb_all, jnp.arange(nb))))


def mla_mixer(h, positions, w_a_down, g_q_lat, g_kv_lat, w_uq, w_ukv, w_oa):
    b, s, _ = h.shape
    lat = h @ w_a_down
    c_q = rms_norm(lat[..., :Q_LORA], g_q_lat)
    c_kv = rms_norm(lat[..., Q_LORA:Q_LORA + KV_LORA], g_kv_lat)
    k_rot = lat[..., Q_LORA + KV_LORA:][:, :, None, :]
    q = (c_q @ w_uq).reshape(b, s, MLA_HEADS, QK_NOPE + QK_ROPE)
    q = jnp.concatenate([q[..., :QK_NOPE], rope(q[..., QK_NOPE:], positions)], axis=-1)
    kv = (c_kv @ w_ukv).reshape(b, s, MLA_HEADS, QK_NOPE + V_DIM)
    k_rot = jnp.broadcast_to(rope(k_rot, positions), (b, s, MLA_HEADS, QK_ROPE))
    k = jnp.concatenate([kv[..., :QK_NOPE], k_rot], axis=-1)
    v = kv[..., QK_NOPE:]
    o = causal_softmax_attention(q, k, v)
    return o.reshape(b, s, MLA_HEADS * V_DIM) @ w_oa


def shared_kv(x, silu_c, w_mod_kv, b_mod_kv, g_kv, w_kv_sb):
    b, s, _ = x.shape
    shift, scale = jnp.split(silu_c @ w_mod_kv + b_mod_kv, 2, axis=-1)
    hk = modulate(rms_norm(x, g_kv), shift, scale)
    kv = (hk @ w_kv_sb).reshape(b, s, 2, SB_HEADS, SB_HEAD_DIM)
    return kv[:, :, 0], kv[:, :, 1]


def stick_breaking_mixer(h, k_sh, v_sh, w_q_sb, w_o_sb):
    b, s, _ = h.shape
    q = (h @ w_q_sb).reshape(b, s, SB_HEADS, SB_HEAD_DIM)
    o = stick_breaking_attention(q, k_sh, v_sh)
    return o.reshape(b, s, SB_HEADS * SB_HEAD_DIM) @ w_o_sb


def swiglu(h, w_gu, w_down):
    gu = h @ w_gu
    return (jax.nn.silu(gu[..., :D_FF]) * gu[..., D_FF:]) @ w_down


def moe_swiglu(h, w_router, b_router, w_exp_gu, w_exp_down):
    b, s, d = h.shape
    hf = h.reshape(b * s, d)
    logits = (hf @ w_router).astype(jnp.float32) + b_router.astype(jnp.float32)
    top_val, top_idx = lax.top_k(logits, TOP_K)
    wts = jax.nn.softmax(top_val, axis=-1)
    comb = jnp.sum(jax.nn.one_hot(top_idx, N_EXPERTS, dtype=h.dtype)
                   * wts[..., None].astype(h.dtype), axis=1)
    y = jnp.zeros_like(hf)
    for e in range(N_EXPERTS):
        y = y + comb[:, e:e + 1] * swiglu(hf, w_exp_gu[e], w_exp_down[e])
    return y.reshape(b, s, d)


def setup_inputs(seed: int = 0) -> dict:
    key = jax.random.key(seed)
    ks = jax.random.split(key, 32)
    D = D_MODEL

    def nrm(k, shape, fan_in, mult=1.0):
        return jax.random.normal(k, shape, jnp.float32) * (mult * fan_in ** -0.5)

    def gain(k, shape):
        return 1.0 + 0.02 * jax.random.normal(k, shape, jnp.float32)

    offsets = jax.random.randint(ks[2], (BATCH, 1), 0, 1024)
    positions = (jnp.arange(SEQ, dtype=jnp.int32)[None, :] + offsets).astype(jnp.int32)
    return {
        "x": jax.random.normal(ks[0], (BATCH, SEQ, D), jnp.float32),
        "c": jax.random.normal(ks[1], (BATCH, D), jnp.float32),
        "positions": positions,
        "w_mod": nrm(ks[3], (DEPTH, D, COND_MULT * D), D, 0.5),
        "b_mod": 0.02 * jax.random.normal(ks[4], (DEPTH, COND_MULT * D), jnp.float32),
        "g_mix": gain(ks[5], (DEPTH, D)),
        "g_ffn": gain(ks[6], (DEPTH, D)),
        "w_a_down": nrm(ks[7], (N_A_LAYERS, D, Q_LORA + KV_LORA + QK_ROPE), D),
        "g_q_lat": gain(ks[8], (N_A_LAYERS, Q_LORA)),
        "g_kv_lat": gain(ks[9], (N_A_LAYERS, KV_LORA)),
        "w_uq": nrm(ks[10], (N_A_LAYERS, Q_LORA, MLA_HEADS * (QK_NOPE + QK_ROPE)), Q_LORA),
        "w_ukv": nrm(ks[11], (N_A_LAYERS, KV_LORA, MLA_HEADS * (QK_NOPE + V_DIM)), KV_LORA),
        "w_oa": nrm(ks[12], (N_A_LAYERS, MLA_HEADS * V_DIM, D), MLA_HEADS * V_DIM),
        "w_mod_kv": nrm(ks[13], (D, 2 * D), D, 0.5),
        "b_mod_kv": 0.02 * jax.random.normal(ks[14], (2 * D,), jnp.float32),
        "g_kv": gain(ks[15], (D,)),
        "w_kv_sb": nrm(ks[16], (D, 2 * SB_HEADS * SB_HEAD_DIM), D),
        "w_q_sb": nrm(ks[17], (N_B_LAYERS, D, SB_HEADS * SB_HEAD_DIM), D),
        "w_o_sb": nrm(ks[18], (N_B_LAYERS, SB_HEADS * SB_HEAD_DIM, D), SB_HEADS * SB_HEAD_DIM),
        "w_ffn_gu": nrm(ks[19], (N_DENSE, D, 2 * D_FF), D),
        "w_ffn_down": nrm(ks[20], (N_DENSE, D_FF, D), D_FF),
        "w_router": nrm(ks[21], (N_MOE, D, N_EXPERTS), D),
        "b_router": 0.01 * jax.random.normal(ks[22], (N_MOE, N_EXPERTS), jnp.float32),
        "w_exp_gu": nrm(ks[23], (N_MOE, N_EXPERTS, D, 2 * D_FF), D),
        "w_exp_down": nrm(ks[24], (N_MOE, N_EXPERTS, D_FF, D), D_FF),
        "g_final": gain(ks[25], (D,)),
    }


def reference(x, c, positions, w_mod, b_mod, g_mix, g_ffn, w_a_down, g_q_lat, g_kv_lat,
              w_uq, w_ukv, w_oa, w_mod_kv, b_mod_kv, g_kv, w_kv_sb, w_q_sb, w_o_sb,
              w_ffn_gu, w_ffn_down, w_router, b_router, w_exp_gu, w_exp_down, g_final):
    silu_c = jax.nn.silu(c)
    k_sh = None
    v_sh = None
    for i in range(DEPTH):
        mod = silu_c @ w_mod[i] + b_mod[i]
        sh1, sc1, gt1, sh2, sc2, gt2 = jnp.split(mod, COND_MULT, axis=-1)
        h = modulate(rms_norm(x, g_mix[i]), sh1, sc1)
        if i < N_A_LAYERS:
            mix = mla_mixer(h, positions, w_a_down[i], g_q_lat[i], g_kv_lat[i],
                            w_uq[i], w_ukv[i], w_oa[i])
        else:
            j = i - N_A_LAYERS
            mix = stick_breaking_mixer(h, k_sh, v_sh, w_q_sb[j], w_o_sb[j])
        x = x + gt1[:, None, :] * mix
        h = modulate(rms_norm(x, g_ffn[i]), sh2, sc2)
        if i % 2 == 0:
            f = swiglu(h, w_ffn_gu[i // 2], w_ffn_down[i // 2])
        else:
            f = moe_swiglu(h, w_router[i // 2], b_router[i // 2],
                           w_exp_gu[i // 2], w_exp_down[i // 2])
        x = x + gt2[:, None, :] * f
        if i == N_A_LAYERS - 1:
            k_sh, v_sh = shared_kv(x, silu_c, w_mod_kv, b_mod_kv, g_kv, w_kv_sb)
    return rms_norm(x, g_final)
```

```python
import math
from contextlib import ExitStack
import numpy as np
import concourse.bass as bass
import concourse.mybir as mybir
from concourse.bass_utils import run_bass_kernel_spmd

F32 = mybir.dt.float32
BF16 = mybir.dt.bfloat16
I32 = mybir.dt.int32
AF = mybir.ActivationFunctionType
ALU = mybir.AluOpType
AX = mybir.AxisListType

EPOCH = 30000
D = 1024
DFF = 3584
NE = 8
EPS = 1e-6
TT = 512
SEQ = 4096
NCORES = 8


class Buf:
    __slots__ = ("ap", "name", "w", "r", "dsem")

    def __init__(self, ap=None, name=""):
        self.ap = ap
        self.name = name
        self.w = None
        self.r = {}
        self.dsem = None


class DSem:
    def __init__(self, prog):
        self.prog = prog
        self.sem = prog.new_sem()
        self.count = 0

    def next(self):
        if self.count + 16 > EPOCH:
            self.sem = self.prog.new_sem()
            self.count = 0
        self.count += 16
        return (self.sem, self.count)


class Op:
    __slots__ = ("eng", "fn", "waits", "needed", "idx", "dma", "tok", "dwaits")

    def __init__(self, eng, fn, dma):
        self.eng = eng
        self.fn = fn
        self.waits = []
        self.dwaits = []
        self.needed = False
        self.idx = None
        self.dma = dma
        self.tok = None


class Prog:
    ENGS = ("pe", "act", "dve", "pool", "sp")

    def __init__(self, nc, es):
        self.nc = nc
        self.es = es
        self.q = {e: [] for e in self.ENGS}
        self.seen = {e: {} for e in self.ENGS}
        self.dseen = {e: {} for e in self.ENGS}
        self.nsem = 0
        self.bufs = []
        self.scratch = None
        self.last_barrier = None

    def new_sem(self):
        s = self.es.enter_context(self.nc.semaphore(f"s{self.nsem}"))
        self.nsem += 1
        return s

    def reg(self, b):
        b.w = self.last_barrier
        self.bufs.append(b)
        return b

    def ps(self, name, shape, dt):
        t = self.es.enter_context(self.nc.psum_tensor(name, list(shape), dt))
        return self.reg(Buf(t, name))

    def dram(self, name, shape, dt, kind=None, reg=True):
        if kind is None:
            t = self.nc.dram_tensor(name, list(shape), dt)
        else:
            t = self.nc.dram_tensor(name, list(shape), dt, kind=kind)
        b = Buf(t.ap(), name)
        return self.reg(b) if reg else b

    def _dep(self, op, d):
        if d is None:
            return
        eng = op.eng
        if d.dma:
            sem, val = d.tok
            k = id(sem)
            if self.dseen[eng].get(k, 0) >= val:
                return
            self.dseen[eng][k] = val
            op.dwaits.append((sem, val))
        else:
            if self.seen[eng].get(d.eng, -1) >= d.idx:
                return
            self.seen[eng][d.eng] = d.idx
            d.needed = True
            op.waits.append(d)

    def emit(self, eng, fn, reads=(), writes=(), dma=False, dsem=None):
        op = Op(eng, fn, dma)
        for b in reads:
            w = b.w
            if w is not None and not (w.eng == eng and eng == "pe" and not w.dma and not dma):
                self._dep(op, w)
        for b in writes:
            w = b.w
            if w is not None and not (w.eng == eng and eng == "pe" and not w.dma and not dma):
                self._dep(op, w)
            for r in b.r.values():
                if (not r.dma) and (not dma) and r.eng == eng and eng == "pe":
                    continue
                self._dep(op, r)
        if dma:
            if dsem is None:
                for b in list(writes) + list(reads):
                    if b.dsem is not None:
                        dsem = b.dsem
                        break
            assert dsem is not None
            op.tok = dsem.next()
        op.idx = len(self.q[eng])
        self.q[eng].append(op)
        for b in writes:
            b.w = op
            b.r = {}
        for b in reads:
            key = id(op.tok[0]) if dma else eng
            b.r[key] = op
        return op

    def dma(self, eng, out, in_, reads=(), writes=(), dsem=None):
        return self.emit(eng, lambda E: E.dma_start(out=out, in_=in_),
                         reads=reads, writes=writes, dma=True, dsem=dsem)

    def barrier(self):
        sc = self.scratch
        self.last_barrier = self.emit("pool", lambda E: E.memset(sc.ap[:, 0:1], 0.0), writes=list(self.bufs))

    def finalize(self, final_waits=()):
        nc = self.nc
        for e in self.ENGS:
            k = 0
            sl = []
            for op in self.q[e]:
                if op.dma:
                    continue
                if op.needed:
                    ep, v = divmod(k, EPOCH)
                    while len(sl) <= ep:
                        sl.append(self.new_sem())
                    op.tok = (sl[ep], v + 1)
                    k += 1
        q = self.q

        def run(E, e):
            for op in q[e]:
                for d in op.waits:
                    E.wait_ge(d.tok[0], d.tok[1])
                for (s, v) in op.dwaits:
                    E.wait_ge(s, v)
                ins = op.fn(E)
                if op.dma:
                    ins.then_inc(op.tok[0], 16)
                elif op.needed:
                    ins.then_inc(op.tok[0], 1)

        with nc.Block() as block:
            @block.tensor
            def _(E):
                run(E, "pe")

            @block.scalar
            def _(E):
                run(E, "act")

            @block.vector
            def _(E):
                run(E, "dve")

            @block.gpsimd
            def _(E):
                run(E, "pool")

            @block.sync
            def _(E):
                run(E, "sp")
                for op in final_waits:
                    E.wait_ge(op.tok[0], op.tok[1])
        return {e: (len(q[e]), sum(1 for o in q[e] if o.needed)) for e in self.ENGS}


def _dtsize(dt):
    return 2 if dt == BF16 else 4


class KB:
    ARENA_KB = 200
    CONST_KB = 12

    def __init__(self, nc, es, S):
        self.nc = nc
        self.S = S
        self.NT = S // TT
        self.NB = S // 128
        P = self.P = Prog(nc, es)
        self.arena = es.enter_context(nc.sbuf_tensor("arena", [128, self.ARENA_KB * 256], F32))
        self.coff = 0
        self.loff = self.CONST_KB * 1024
        self.free_dsems = []
        self.local_dsems = []
        P.scratch = self.calloc("scratch", [128, 16], F32)
        self.psb = [P.ps(f"psb{i}", [128, 512], F32) for i in range(8)]
        self.psg = self.psb[0:4]
        self.psa = self.psb[4:6]
        self.pst = [P.reg(Buf(self.psb[6 + i].ap[:, :].bitcast(BF16), f"pst{i}")) for i in range(2)]
        self.psg_i = 0
        self.out_ops = []

    def _view(self, off, shape, dt):
        n = 1
        for s in shape[1:]:
            n *= s
        nb = n * _dtsize(dt)
        assert off % 4 == 0 and nb % 4 == 0
        a = self.arena[:, off // 4: (off + nb) // 4]
        if dt != F32:
            a = a.bitcast(dt)
        if len(shape) == 3:
            a = a.rearrange("p (a b) -> p a b", a=shape[1])
        elif len(shape) == 4:
            a = a.rearrange("p (a b c) -> p a b c", a=shape[1], b=shape[2])
        if shape[0] != 128:
            a = a[0:shape[0]]
        return a, nb

    def calloc(self, name, shape, dt, dma=False):
        a, nb = self._view(self.coff, shape, dt)
        self.coff += (nb + 63) // 64 * 64
        assert self.coff <= self.CONST_KB * 1024, (name, self.coff)
        b = self.P.reg(Buf(a, name))
        if dma:
            b.dsem = DSem(self.P)
        return b

    def lalloc(self, name, shape, dt, dma=False):
        a, nb = self._view(self.loff, shape, dt)
        self.loff += (nb + 63) // 64 * 64
        assert self.loff <= self.ARENA_KB * 1024, (name, self.loff)
        b = self.P.reg(Buf(a, name))
        if dma:
            if self.free_dsems:
                b.dsem = self.free_dsems.pop()
            else:
                b.dsem = DSem(self.P)
            self.local_dsems.append(b.dsem)
        return b

    def phase_end(self):
        self.P.barrier()
        self.loff = self.CONST_KB * 1024
        self.free_dsems.extend(self.local_dsems)
        self.local_dsems = []

    def next_ps(self):
        b = self.psg[self.psg_i % 4]
        self.psg_i += 1
        return b

    def mm(self, out, lhsT, rhs, start, stop, reads, writes):
        self.P.emit("pe", lambda E: E.matmul(out, lhsT, rhs, start=start, stop=stop), reads=reads, writes=writes)

    def act(self, out, in_, func, reads, writes, bias=0.0, scale=1.0, accum=None):
        if accum is None:
            self.P.emit("act", lambda E: E.activation(out, in_, func, bias=bias, scale=scale), reads=reads, writes=writes)
        else:
            self.P.emit("act", lambda E: E.activation(out, in_, func, bias=bias, scale=scale, accum_out=accum), reads=reads, writes=writes)

    def copy(self, eng, out, in_, reads, writes):
        if eng == "act":
            self.P.emit("act", lambda E: E.copy(out, in_), reads=reads, writes=writes)
        else:
            self.P.emit(eng, lambda E: E.tensor_copy(out, in_), reads=reads, writes=writes)

    def tt(self, eng, out, in0, in1, op, reads, writes):
        self.P.emit(eng, lambda E: E.tensor_tensor(out, in0, in1, op), reads=reads, writes=writes)

    def ts(self, eng, out, in0, s1, s2, op0, op1, reads, writes):
        if op1 is None:
            self.P.emit(eng, lambda E: E.tensor_scalar(out, in0, s1, None, op0), reads=reads, writes=writes)
        else:
            self.P.emit(eng, lambda E: E.tensor_scalar(out, in0, s1, s2, op0, op1), reads=reads, writes=writes)

    def stt(self, eng, out, in0, scalar, in1, op0, op1, reads, writes):
        self.P.emit(eng, lambda E: E.scalar_tensor_tensor(out, in0, scalar, in1, op0, op1), reads=reads, writes=writes)

    def wload(self, wd, K, c0, ncols, r0=0):
        KC = K // 128
        wb = self.wbufs[self.wb_i % len(self.wbufs)]
        self.wb_i += 1
        v = wb.ap[:, 0:KC * ncols].rearrange("p (c n) -> p c n", c=KC)
        src = wd.ap[r0:r0 + K, c0:c0 + ncols].rearrange("(c p) n -> p c n", p=128)
        self.P.dma("sp", v, src, reads=[wd], writes=[wb])
        return wb, v

    def lin_fm(self, wd, K, c0, ncols, rhs, rhs_bufs, N, consume, group=512, r0=0):
        KC = K // 128
        for g0 in range(c0, c0 + ncols, group):
            gw = min(group, c0 + ncols - g0)
            wb, v = self.wload(wd, K, g0, gw, r0=r0)
            for j in range(gw // 128):
                ps = self.next_ps()
                for kc in range(KC):
                    self.mm(ps.ap[:, :N], v[:, kc, j * 128:(j + 1) * 128], rhs(kc), kc == 0, kc == KC - 1,
                            [wb] + rhs_bufs, [ps])
                consume((g0 - c0) // 128 + j, ps)

    def lin_tm(self, wd, K, c0, ncols, lhs, lhs_bufs, nblk, consume, group=512):
        KC = K // 128
        for g0 in range(c0, c0 + ncols, group):
            gw = min(group, c0 + ncols - g0)
            wb, v = self.wload(wd, K, g0, gw)
            for blk in range(nblk):
                ps = self.next_ps()
                for kc in range(KC):
                    self.mm(ps.ap[:, :gw], lhs(kc, blk), v[:, kc, :], kc == 0, kc == KC - 1, [wb] + lhs_bufs, [ps])
                consume(blk, g0 - c0, gw, ps)

    def norm_mod(self, x, nch, nfeat, scale_ap, bias_ap, outs):
        P = self.P
        ps = self.next_ps()
        for c in range(nch):
            sq = self.tmp()
            self.act(sq.ap[:, :], x.ap[:, c, :], AF.Square, [x], [sq])
            self.mm(ps.ap[:, :], self.ones_f.ap[:, :], sq.ap[:, :], c == 0, c == nch - 1, [self.ones_f, sq], [ps])
        rstd = self.rstd_t[self.rstd_i % 2]
        self.rstd_i += 1
        self.ts("dve", rstd.ap[:, :], ps.ap[:, :], 1.0 / nfeat, EPS, ALU.mult, ALU.add, [ps], [rstd])
        self.act(rstd.ap[:, :], rstd.ap[:, :], AF.Sqrt, [rstd], [rstd])
        P.emit("dve", lambda E: E.reciprocal(rstd.ap[:, :], rstd.ap[:, :]), reads=[rstd], writes=[rstd])
        for c in range(nch):
            t = self.tmp()
            self.tt("dve", t.ap[:, :], x.ap[:, c, :], rstd.ap[:, :], ALU.mult, [x, rstd], [t])
            for (o, sbufs) in outs:
                b = bias_ap(c) if bias_ap is not None else 0.0
                self.act(o.ap[:, c, :], t.ap[:, :], AF.Identity, [t] + sbufs, [o], bias=b, scale=scale_ap(c))

    def tmp(self):
        t = self.tmps[self.tmp_i % len(self.tmps)]
        self.tmp_i += 1
        return t

    def build(self):
        P, S, NT, NB = self.P, self.S, self.NT, self.NB
        nc = self.nc
        xT = P.dram("xT", [D, S], F32, kind="ExternalInput")
        cvec = P.dram("cvec", [128, 8], F32, kind="ExternalInput")
        pos = P.dram("pos", [1, S], I32, kind="ExternalInput")
        ropec = P.dram("ropec", [128, 2], F32, kind="ExternalInput")
        w_mod = P.dram("w_mod", [2, D, 6 * D], F32, kind="ExternalInput")
        w_mod_kv = P.dram("w_mod_kv", [D, 2 * D], F32, kind="ExternalInput")
        bmod = P.dram("bmod", [128, 112], F32, kind="ExternalInput")
        gvec = P.dram("gvec", [128, 48], F32, kind="ExternalInput")
        glat = P.dram("glat", [128, 5], F32, kind="ExternalInput")
        brout = P.dram("brout", [1, NE], F32, kind="ExternalInput")
        wrout = P.dram("wrout", [D, NE], F32, kind="ExternalInput")
        wf = {}
        wshape = {"wad": [D, 896], "wuq": [384, 2048], "wukv": [256, 2048], "woa": [D, D],
                  "wgu": [D, 2 * DFF], "wdn": [DFF, D], "wkvsb": [D, 2 * D], "wqsb": [D, D], "wosb": [D, D],
                  "wegu": [NE * D, 2 * DFF], "wedn": [NE * DFF, D]}
        worder = ["wad", "wuq", "wukv", "woa", "wgu", "wdn", "wkvsb", "wqsb", "wosb", "wegu", "wedn"]
        for n in worder:
            wf[n] = P.dram(n, wshape[n], F32, kind="ExternalInput", reg=False)
        outT = P.dram("outT", [D, S], F32, kind="ExternalOutput")
        wb16 = {n: P.dram(n + "_b", wshape[n], BF16, reg=False) for n in worder}
        for n in worder:
            wb16[n].dsem = DSem(P)
        qn_s = P.dram("qn_s", [8, 128, S], BF16)
        qr_s = P.dram("qr_s", [4, 128, S], BF16)
        kn_s = P.dram("kn_s", [8, 128, S], BF16)
        kr_s = P.dram("kr_s", [128, S], BF16)
        v_s = P.dram("v_s", [S, D], BF16)
        ot_s = P.dram("ot_s", [D, S], BF16)
        x2_s = P.dram("x2_s", [D, S], F32)
        ksb_s = P.dram("ksb_s", [8, 128, S], BF16)
        vsb_s = P.dram("vsb_s", [S, D], BF16)
        qsb_s = P.dram("qsb_s", [8, 128, S], BF16)
        ot2_s = P.dram("ot2_s", [D, S], BF16)

        def cast_weights(names):
            for n in names:
                rows = wshape[n][0]
                step = 128
                for r in range(0, rows, step):
                    P.dma("pool", wb16[n].ap[r:r + step, :], wf[n].ap[r:r + step, :], reads=[wf[n]], writes=[wb16[n]],
                          dsem=wb16[n].dsem)

        ident_f = self.calloc("ident_f", [128, 128], F32)
        self.ones_f = self.calloc("ones_f", [128, 128], F32)
        ident_b = self.calloc("ident_b", [128, 128], BF16)
        triU = self.calloc("triU", [128, 128], BF16)
        triL = self.calloc("triL", [128, 128], BF16)
        cmask = self.calloc("cmask", [128, 128], F32)
        m01 = [self.calloc(f"m01_{o}", [128, 512], BF16) for o in range(4)]
        silc = self.calloc("silc", [128, 8], F32, dma=True)
        modv = self.calloc("modv", [128, 112], F32)
        bmod_t = self.calloc("bmod_t", [128, 112], F32, dma=True)
        gvec_t = self.calloc("gvec_t", [128, 48], F32, dma=True)
        glat_t = self.calloc("glat_t", [128, 5], F32, dma=True)
        ropec_t = self.calloc("ropec_t", [128, 2], F32, dma=True)
        brout_t = self.calloc("brout_t", [128, NE], F32, dma=True)
        wrout_t = self.calloc("wrout_t", [128, 8, NE], F32, dma=True)
        avec = self.calloc("avec", [128, 5, 8], F32)
        tmpc = self.calloc("tmpc", [128, 128], F32)

        def cset(buf, pattern, cm, base, cmp, fill, val=1.0):
            P.emit("pool", lambda E: E.memset(tmpc.ap[:, :], val), writes=[tmpc])
            P.emit("pool", lambda E: E.affine_select(tmpc.ap[:, :], tmpc.ap[:, :], pattern, cmp, fill, base=base,
                                                     channel_multiplier=cm), reads=[tmpc], writes=[tmpc])
            self.copy("pool", buf.ap[:, :], tmpc.ap[:, :], [tmpc], [buf])

        cset(ident_f, [[-1, 128]], 1, 0, ALU.is_equal, 0.0)
        cset(ident_b, [[-1, 128]], 1, 0, ALU.is_equal, 0.0)
        cset(triU, [[-1, 128]], 1, 0, ALU.is_ge, 0.0)
        cset(triL, [[1, 128]], -1, 0, ALU.is_gt, 0.0)
        cset(cmask, [[-1, 128]], 1, 0, ALU.is_ge, -1.0e9, val=0.0)
        P.emit("pool", lambda E: E.memset(self.ones_f.ap[:, :], 1.0), writes=[self.ones_f])

        cast_weights(["wad", "wuq", "wukv"])
        P.dma("sp", silc.ap[:, :], cvec.ap[:, :], reads=[cvec], writes=[silc])
        P.dma("sp", bmod_t.ap[:, :], bmod.ap[:, :], reads=[bmod], writes=[bmod_t])
        P.dma("sp", gvec_t.ap[:, :], gvec.ap[:, :], reads=[gvec], writes=[gvec_t])
        P.dma("sp", glat_t.ap[:, :], glat.ap[:, :], reads=[glat], writes=[glat_t])
        P.dma("sp", ropec_t.ap[:, :], ropec.ap[:, :], reads=[ropec], writes=[ropec_t])
        P.dma("sp", brout_t.ap[:, :], brout.ap[0:1, :].partition_broadcast(128), reads=[brout], writes=[brout_t])
        P.dma("sp", wrout_t.ap[:, :, :], wrout.ap.rearrange("(c p) e -> p c e", p=128), reads=[wrout], writes=[wrout_t])
        self.act(silc.ap[:, :], silc.ap[:, :], AF.Silu, [silc], [silc])

        cosT = self.lalloc("cosT", [128, S], F32)
        sinT = self.lalloc("sinT", [128, S], F32)
        m01f = self.lalloc("m01f", [128, 512], F32)
        for o in range(4):
            P.emit("pool", lambda E: E.memset(m01f.ap[:, :], 1.0), writes=[m01f])
            P.emit("pool", lambda E, o=o: E.affine_select(m01f.ap[:, :], m01f.ap[:, :], [[1, 512]], ALU.is_gt, 0.0,
                                                          base=-128 * o, channel_multiplier=-1), reads=[m01f], writes=[m01f])
            self.copy("pool", m01[o].ap[:, :], m01f.ap[:, :], [m01f], [m01[o]])
        wmb = [self.lalloc(f"wmb{i}", [128, 8, 512], F32, dma=True) for i in range(2)]
        psm = self.psa[0]
        gi = 0
        for (wsrc, ncol, cbase) in ((w_mod.ap[0], 6 * D, 0), (w_mod.ap[1], 6 * D, 48), (w_mod_kv.ap, 2 * D, 96)):
            for g0 in range(0, ncol, 512):
                wb = wmb[gi % 2]
                gi += 1
                P.dma("sp", wb.ap[:, :, :], wsrc[:, g0:g0 + 512].rearrange("(c p) n -> p c n", p=128),
                      reads=[w_mod, w_mod_kv], writes=[wb])
                for j in range(4):
                    col = cbase + g0 // 128 + j
                    for kc in range(8):
                        self.mm(psm.ap[:, col:col + 1], wb.ap[:, kc, j * 128:(j + 1) * 128], silc.ap[:, kc:kc + 1],
                                kc == 0, kc == 7, [wb, silc], [psm])
        self.tt("dve", modv.ap[:, :], psm.ap[:, 0:112], bmod_t.ap[:, :], ALU.add, [psm, bmod_t], [modv])

        def mk_a(slot, gcol, sccol):
            self.stt("dve", avec.ap[:, slot, :], modv.ap[:, sccol:sccol + 8], 1.0, gvec_t.ap[:, gcol:gcol + 8],
                     ALU.add, ALU.mult, [modv, gvec_t], [avec])
        mk_a(0, 0, 8)
        mk_a(1, 16, 32)
        mk_a(2, 8, 48 + 8)
        mk_a(3, 24, 48 + 32)
        mk_a(4, 32, 104)

        TWO_PI = float(2 * np.pi)
        PI = float(np.pi)
        posi = self.lalloc("posi", [128, S], I32, dma=True)
        ang = self.lalloc("ang", [128, S], F32)
        pt = self.lalloc("pt", [128, S], F32)
        pti = self.lalloc("pti", [128, S], I32)
        P.dma("sp", posi.ap[:, :], pos.ap[0:1, :].partition_broadcast(128), reads=[pos], writes=[posi])
        self.copy("dve", ang.ap[:, :], posi.ap[:, :], [posi], [ang])
        self.ts("dve", ang.ap[:, :], ang.ap[:, :], ropec_t.ap[:, 0:1], None, ALU.mult, None, [ang, ropec_t], [ang])
        for (dst, shift) in ((sinT, 0.0), (cosT, PI / 2)):
            r = dst
            self.ts("dve", r.ap[:, :], ang.ap[:, :], shift, None, ALU.add, None, [ang], [r])
            self.ts("dve", pt.ap[:, :], r.ap[:, :], 1.0 / TWO_PI, 0.5, ALU.mult, ALU.add, [r], [pt])
            self.copy("dve", pti.ap[:, :], pt.ap[:, :], [pt], [pti])
            self.copy("dve", pt.ap[:, :], pti.ap[:, :], [pti], [pt])
            self.stt("dve", r.ap[:, :], pt.ap[:, :], -TWO_PI, r.ap[:, :], ALU.mult, ALU.add, [pt, r], [r])
            self.ts("dve", pt.ap[:, :], r.ap[:, :], PI, -TWO_PI, ALU.is_gt, ALU.mult, [r], [pt])
            self.tt("dve", r.ap[:, :], r.ap[:, :], pt.ap[:, :], ALU.add, [r, pt], [r])
            self.ts("dve", pt.ap[:, :], r.ap[:, :], -PI, TWO_PI, ALU.is_lt, ALU.mult, [r], [pt])
            self.tt("dve", r.ap[:, :], r.ap[:, :], pt.ap[:, :], ALU.add, [r, pt], [r])
            self.ts("dve", r.ap[:, :], r.ap[:, :], PI, -PI, ALU.min, ALU.max, [r], [r])
            self.act(r.ap[:, :], r.ap[:, :], AF.Sin, [r], [r])
        self.ts("dve", sinT.ap[:, :], sinT.ap[:, :], ropec_t.ap[:, 1:2], None, ALU.mult, None, [sinT, ropec_t], [sinT])
        P.barrier()
        self.loff = self.CONST_KB * 1024 + 2 * S * 4
        self.free_dsems.extend(self.local_dsems)
        self.local_dsems = []
        cast_weights(["woa", "wgu", "wdn", "wkvsb", "wqsb", "wosb"])

        def tile_bufs(nw=3):
            self.wbufs = [self.lalloc(f"wb{i}", [128, 7168], BF16, dma=True) for i in range(nw)]
            self.wb_i = 0
            self.tmps = [self.lalloc(f"tmp{i}", [128, 512], F32) for i in range(3)]
            self.tmp_i = 0
            self.rstd_t = [self.lalloc(f"rstd{i}", [128, 512], F32) for i in range(2)]
            self.rstd_i = 0

        tile_bufs()
        xf = [self.lalloc(f"xf{i}", [128, 8, TT], F32, dma=True) for i in range(2)]
        hb = self.lalloc("hb", [128, 8, TT], BF16)
        latq = self.lalloc("latq", [128, 3, TT], F32)
        latkv = self.lalloc("latkv", [128, 2, TT], F32)
        ropeA = self.lalloc("ropeA", [128, TT], F32)
        ropeB = self.lalloc("ropeB", [128, TT], F32)
        cq = self.lalloc("cq", [128, 3, TT], BF16)
        ckv = self.lalloc("ckv", [128, 2, TT], BF16)
        krst = self.lalloc("krst", [128, TT], BF16, dma=True)
        qnst = self.lalloc("qnst", [128, 8, TT], BF16, dma=True)
        qrst = self.lalloc("qrst", [128, 4, TT], BF16, dma=True)
        knst = self.lalloc("knst", [128, 8, TT], BF16, dma=True)
        vst = self.lalloc("vst", [128, 4, D], BF16, dma=True)
        qA = self.lalloc("qA", [128, 4, TT], F32)

        def rope_combine(dst_ap, A_ap, B_ap, t0, reads, dst_buf):
            t1 = self.tmp()
            self.tt("dve", t1.ap[:, :], A_ap, cosT.ap[:, t0:t0 + TT], ALU.mult, reads + [cosT], [t1])
            t2 = self.tmp()
            self.tt("dve", t2.ap[:, :], B_ap, sinT.ap[:, t0:t0 + TT], ALU.mult, reads + [sinT], [t2])
            self.tt("dve", dst_ap, t1.ap[:, :], t2.ap[:, :], ALU.add, [t1, t2], [dst_buf])

        xTv = xT.ap.rearrange("(c p) s -> p c s", p=128)
        for t in range(NT):
            t0 = t * TT
            x = xf[t % 2]
            P.dma("sp", x.ap[:, :, :], xTv[:, :, t0:t0 + TT], reads=[xT], writes=[x])
            self.norm_mod(x, 8, D, lambda c: avec.ap[:, 0, c:c + 1], lambda c: modv.ap[:, c:c + 1],
                          [(hb, [avec, modv])])

            def lat_consume(oc, ps, t0=t0):
                if oc < 3:
                    self.copy("act", latq.ap[:, oc, :], ps.ap[:, :], [ps], [latq])
                elif oc < 5:
                    self.copy("act", latkv.ap[:, oc - 3, :], ps.ap[:, :], [ps], [latkv])
                elif oc == 5:
                    self.copy("act", ropeA.ap[:, :], ps.ap[:, :], [ps], [ropeA])
                else:
                    self.copy("act", ropeB.ap[:, :], ps.ap[:, :], [ps], [ropeB])
            self.lin_fm(wb16["wad"], D, 0, 896, lambda kc: hb.ap[:, kc, :], [hb], TT, lat_consume)
            rope_combine(krst.ap[:, :], ropeA.ap[:, :], ropeB.ap[:, :], t0, [ropeA, ropeB], krst)
            P.dma("act", kr_s.ap[:, t0:t0 + TT], krst.ap[:, :], reads=[krst], writes=[kr_s])
            self.norm_mod(latq, 3, 384, lambda c: glat_t.ap[:, c:c + 1], None, [(cq, [glat_t])])
            self.norm_mod(latkv, 2, 256, lambda c: glat_t.ap[:, 3 + c:4 + c], None, [(ckv, [glat_t])])

            def q_consume(oc, ps, t0=t0):
                if oc < 8:
                    self.copy("act", qnst.ap[:, oc, :], ps.ap[:, :], [ps], [qnst])
                elif oc < 12:
                    self.copy("act", qA.ap[:, oc - 8, :], ps.ap[:, :], [ps], [qA])
                else:
                    j = oc - 12
                    t1 = self.tmp()
                    self.tt("dve", t1.ap[:, :], qA.ap[:, j, :], cosT.ap[:, t0:t0 + TT], ALU.mult, [qA, cosT], [t1])
                    t2 = self.tmp()
                    self.tt("dve", t2.ap[:, :], ps.ap[:, :], sinT.ap[:, t0:t0 + TT], ALU.mult, [ps, sinT], [t2])
                    self.tt("dve", qrst.ap[:, j, :], t1.ap[:, :], t2.ap[:, :], ALU.add, [t1, t2], [qrst])
            self.lin_fm(wb16["wuq"], 384, 0, 2048, lambda kc: cq.ap[:, kc, :], [cq], TT, q_consume)
            P.dma("act", qn_s.ap[:, :, t0:t0 + TT].rearrange("h p s -> p h s"), qnst.ap[:, :, :], reads=[qnst], writes=[qn_s])
            P.dma("act", qr_s.ap[:, :, t0:t0 + TT].rearrange("h p s -> p h s"), qrst.ap[:, :, :], reads=[qrst], writes=[qr_s])

            def kn_consume(oc, ps):
                self.copy("act", knst.ap[:, oc, :], ps.ap[:, :], [ps], [knst])
            self.lin_fm(wb16["wukv"], 256, 0, 1024, lambda kc: ckv.ap[:, kc, :], [ckv], TT, kn_consume)
            P.dma("act", kn_s.ap[:, :, t0:t0 + TT].rearrange("h p s -> p h s"), knst.ap[:, :, :], reads=[knst], writes=[kn_s])

            def v_consume(blk, c0, gw, ps):
                self.copy("act", vst.ap[:, blk, c0:c0 + gw], ps.ap[:, :gw], [ps], [vst])
            self.lin_tm(wb16["wukv"], 256, 1024, 1024, lambda kc, blk: ckv.ap[:, kc, blk * 128:(blk + 1) * 128], [ckv], 4, v_consume)
            P.dma("act", v_s.ap[t0:t0 + TT, :].rearrange("(b p) n -> p b n", p=128), vst.ap[:, :, :], reads=[vst], writes=[v_s])
        self.phase_end()
        cast_weights(["wegu", "wedn"])

        self.attn_mla(qn_s, qr_s, kn_s, kr_s, v_s, ot_s, ident_b, cmask)
        self.phase_end()

        tile_bufs()
        xf = [self.lalloc(f"xf{i}", [128, 8, TT], F32, dma=True) for i in range(2)]
        otl = [self.lalloc(f"otl{i}", [128, 8, TT], BF16, dma=True) for i in range(2)]
        hb = self.lalloc("hb", [128, 8, TT], BF16)
        actb = self.lalloc("actb", [128, 28, TT], BF16)
        kst = self.lalloc("kst", [128, 8, TT], BF16, dma=True)
        vst = self.lalloc("vst", [128, 4, D], BF16, dma=True)
        qst = self.lalloc("qst", [128, 8, TT], BF16, dma=True)
        otv = ot_s.ap.rearrange("(c p) s -> p c s", p=128)
        x2v = x2_s.ap.rearrange("(c p) s -> p c s", p=128)

        def resid_consume(x, gcol):
            def f(oc, ps):
                self.stt("dve", x.ap[:, oc, :], ps.ap[:, :], modv.ap[:, gcol + oc:gcol + oc + 1], x.ap[:, oc, :],
                         ALU.mult, ALU.add, [ps, modv, x], [x])
            return f

        def ffn(wgu_d, wdn_d, r0gu, r0dn, hb, down_consume, comb=None):
            for c2 in range(0, 28, 2):
                wb = self.wbufs[self.wb_i % len(self.wbufs)]
                self.wb_i += 1
                v = wb.ap[:, 0:8 * 512].rearrange("p (c n) -> p c n", c=8)
                P.dma("sp", v, wgu_d.ap[r0gu:r0gu + D, (c2 // 2) * 512:(c2 // 2) * 512 + 512].rearrange("(c p) n -> p c n", p=128),
                      reads=[wgu_d], writes=[wb])
                for j in range(2):
                    pg = self.next_ps()
                    for kc in range(8):
                        self.mm(pg.ap[:, :], v[:, kc, j * 128:(j + 1) * 128], hb.ap[:, kc, :], kc == 0, kc == 7, [wb, hb], [pg])
                    pu = self.next_ps()
                    for kc in range(8):
                        self.mm(pu.ap[:, :], v[:, kc, 256 + j * 128:256 + (j + 1) * 128], hb.ap[:, kc, :], kc == 0, kc == 7, [wb, hb], [pu])
                    sg = self.tmp()
                    self.act(sg.ap[:, :], pg.ap[:, :], AF.Silu, [pg], [sg])
                    if comb is None:
                        self.tt("dve", actb.ap[:, c2 + j, :], sg.ap[:, :], pu.ap[:, :], ALU.mult, [sg, pu], [actb])
                    else:
                        s2 = self.tmp()
                        self.tt("dve", s2.ap[:, :], sg.ap[:, :], pu.ap[:, :], ALU.mult, [sg, pu], [s2])
                        self.tt("dve", actb.ap[:, c2 + j, :], s2.ap[:, :], comb[0], ALU.mult, [s2, comb[1]], [actb])
            self.lin_fm(wdn_d, DFF, 0, D, lambda kc: actb.ap[:, kc, :], [actb], TT, down_consume, group=256, r0=r0dn)

        for t in range(NT):
            t0 = t * TT
            x = xf[t % 2]
            ot = otl[t % 2]
            P.dma("sp", x.ap[:, :, :], xTv[:, :, t0:t0 + TT], reads=[xT], writes=[x])
            P.dma("sp", ot.ap[:, :, :], otv[:, :, t0:t0 + TT], reads=[ot_s], writes=[ot])
            self.lin_fm(wb16["woa"], D, 0, D, lambda kc: ot.ap[:, kc, :], [ot], TT, resid_consume(x, 16))
            self.norm_mod(x, 8, D, lambda c: avec.ap[:, 1, c:c + 1], lambda c: modv.ap[:, 24 + c:25 + c], [(hb, [avec, modv])])
            ffn(wb16["wgu"], wb16["wdn"], 0, 0, hb, resid_consume(x, 40))
            P.dma("act", x2v[:, :, t0:t0 + TT], x.ap[:, :, :], reads=[x], writes=[x2_s])
            self.norm_mod(x, 8, D, lambda c: avec.ap[:, 4, c:c + 1], lambda c: modv.ap[:, 96 + c:97 + c], [(hb, [avec, modv])])

            def k_consume(oc, ps):
                self.copy("act", kst.ap[:, oc, :], ps.ap[:, :], [ps], [kst])
            self.lin_fm(wb16["wkvsb"], D, 0, D, lambda kc: hb.ap[:, kc, :], [hb], TT, k_consume)
            P.dma("act", ksb_s.ap[:, :, t0:t0 + TT].rearrange("h p s -> p h s"), kst.ap[:, :, :], reads=[kst], writes=[ksb_s])

            def v_consume(blk, c0, gw, ps):
                self.copy("act", vst.ap[:, blk, c0:c0 + gw], ps.ap[:, :gw], [ps], [vst])
            self.lin_tm(wb16["wkvsb"], D, D, D, lambda kc, blk: hb.ap[:, kc, blk * 128:(blk + 1) * 128], [hb], 4, v_consume)
            P.dma("act", vsb_s.ap[t0:t0 + TT, :].rearrange("(b p) n -> p b n", p=128), vst.ap[:, :, :], reads=[vst], writes=[vsb_s])
            self.norm_mod(x, 8, D, lambda c: avec.ap[:, 2, c:c + 1], lambda c: modv.ap[:, 48 + c:49 + c], [(hb, [avec, modv])])

            def q_consume(oc, ps):
                self.copy("act", qst.ap[:, oc, :], ps.ap[:, :], [ps], [qst])
            self.lin_fm(wb16["wqsb"], D, 0, D, lambda kc: hb.ap[:, kc, :], [hb], TT, q_consume)
            P.dma("act", qsb_s.ap[:, :, t0:t0 + TT].rearrange("h p s -> p h s"), qst.ap[:, :, :], reads=[qst], writes=[qsb_s])
        self.phase_end()

        self.attn_sb(qsb_s, ksb_s, vsb_s, ot2_s, triU, triL, m01)
        self.phase_end()

        tile_bufs()
        xf = [self.lalloc(f"xf{i}", [128, 8, TT], F32, dma=True) for i in range(2)]
        otl = [self.lalloc("otl0", [128, 8, TT], BF16, dma=True)] * 2
        hb = self.lalloc("hb", [128, 8, TT], BF16)
        hf = self.lalloc("hf", [128, 8, TT], F32, dma=True)
        actb = self.lalloc("actb", [128, 28, TT], BF16)
        yacc = self.lalloc("yacc", [128, 8, TT], F32)
        combB = self.lalloc("combB", [128, NE, TT], F32)
        outst = hf
        lg = self.lalloc("lg", [128, 4, NE], F32)
        rt = [self.lalloc(f"rt{i}", [128, 4, NE], F32) for i in range(4)]
        rs = [self.lalloc(f"rs{i}", [128, 4], F32) for i in range(6)]
        dg = [self.lalloc(f"dg{i}", [128, 128], F32) for i in range(3)]
        ot2v = ot2_s.ap.rearrange("(c p) s -> p c s", p=128)
        outv = outT.ap.rearrange("(c p) s -> p c s", p=128)
        dgi = 0
        for t in range(NT):
            t0 = t * TT
            x = xf[t % 2]
            ot = otl[t % 2]
            P.dma("sp", x.ap[:, :, :], x2v[:, :, t0:t0 + TT], reads=[x2_s], writes=[x])
            P.dma("sp", ot.ap[:, :, :], ot2v[:, :, t0:t0 + TT], reads=[ot2_s], writes=[ot])
            self.lin_fm(wb16["wosb"], D, 0, D, lambda kc: ot.ap[:, kc, :], [ot], TT, resid_consume(x, 48 + 16))
            self.norm_mod(x, 8, D, lambda c: avec.ap[:, 3, c:c + 1], lambda c: modv.ap[:, 48 + 24 + c:48 + 25 + c],
                          [(hb, [avec, modv]), (hf, [avec, modv])])
            for blk in range(4):
                ps = self.next_ps()
                for kc in range(8):
                    self.mm(ps.ap[:, 0:NE], hf.ap[:, kc, blk * 128:(blk + 1) * 128], wrout_t.ap[:, kc, :], kc == 0, kc == 7,
                            [hf, wrout_t], [ps])
                self.tt("dve", lg.ap[:, blk, :], ps.ap[:, 0:NE], brout_t.ap[:, :], ALU.add, [ps, brout_t], [lg])
            m1, m2, dd, ee, w1, w2 = rs
            eq1, lg2, eq2, comb = rt
            P.emit("dve", lambda E: E.tensor_reduce(m1.ap[:, :], lg.ap[:, :, :], AX.X, ALU.max), reads=[lg], writes=[m1])
            self.tt("dve", eq1.ap[:, :, :], lg.ap[:, :, :], m1.ap[:, :].unsqueeze(2).to_broadcast([128, 4, NE]), ALU.is_equal, [lg, m1], [eq1])
            self.stt("dve", lg2.ap[:, :, :], eq1.ap[:, :, :], -1.0e30, lg.ap[:, :, :], ALU.mult, ALU.add, [eq1, lg], [lg2])
            P.emit("dve", lambda E: E.tensor_reduce(m2.ap[:, :], lg2.ap[:, :, :], AX.X, ALU.max), reads=[lg2], writes=[m2])
            self.tt("dve", eq2.ap[:, :, :], lg2.ap[:, :, :], m2.ap[:, :].unsqueeze(2).to_broadcast([128, 4, NE]), ALU.is_equal, [lg2, m2], [eq2])
            self.tt("dve", dd.ap[:, :], m2.ap[:, :], m1.ap[:, :], ALU.subtract, [m1, m2], [dd])
            self.act(ee.ap[:, :], dd.ap[:, :], AF.Exp, [dd], [ee])
            self.ts("dve", w1.ap[:, :], ee.ap[:, :], 1.0, None, ALU.add, None, [ee], [w1])
            P.emit("dve", lambda E: E.reciprocal(w1.ap[:, :], w1.ap[:, :]), reads=[w1], writes=[w1])
            self.tt("dve", w2.ap[:, :], ee.ap[:, :], w1.ap[:, :], ALU.mult, [ee, w1], [w2])
            self.tt("dve", eq1.ap[:, :, :], eq1.ap[:, :, :], w1.ap[:, :].unsqueeze(2).to_broadcast([128, 4, NE]), ALU.mult, [eq1, w1], [eq1])
            self.tt("dve", eq2.ap[:, :, :], eq2.ap[:, :, :], w2.ap[:, :].unsqueeze(2).to_broadcast([128, 4, NE]), ALU.mult, [eq2, w2], [eq2])
            self.tt("dve", comb.ap[:, :, :], eq1.ap[:, :, :], eq2.ap[:, :, :], ALU.add, [eq1, eq2], [comb])
            for e in range(NE):
                ps = self.next_ps()
                for blk in range(4):
                    d = dg[dgi % 3]
                    dgi += 1
                    self.ts("dve", d.ap[:, :], ident_f.ap[:, :], comb.ap[:, blk, e:e + 1], None, ALU.mult, None, [ident_f, comb], [d])
                    self.mm(ps.ap[:, blk * 128:(blk + 1) * 128], self.ones_f.ap[:, :], d.ap[:, :], True, True, [self.ones_f, d], [ps])
                self.copy("act", combB.ap[:, e, :], ps.ap[:, :], [ps], [combB])
            for e in range(NE):
                def y_consume(oc, ps, e=e):
                    if e == 0:
                        self.copy("dve", yacc.ap[:, oc, :], ps.ap[:, :], [ps], [yacc])
                    else:
                        self.tt("dve", yacc.ap[:, oc, :], yacc.ap[:, oc, :], ps.ap[:, :], ALU.add, [ps, yacc], [yacc])
                ffn(wb16["wegu"], wb16["wedn"], e * D, e * DFF, hb, y_consume, comb=(combB.ap[:, e, :], combB))
            for oc in range(8):
                self.stt("dve", x.ap[:, oc, :], yacc.ap[:, oc, :], modv.ap[:, 48 + 40 + oc:48 + 41 + oc], x.ap[:, oc, :],
                         ALU.mult, ALU.add, [yacc, modv, x], [x])
            self.norm_mod(x, 8, D, lambda c: gvec_t.ap[:, 40 + c:41 + c], None, [(outst, [gvec_t])])
            op = P.dma("act", outv[:, :, t0:t0 + TT], outst.ap[:, :, :], reads=[outst], writes=[outT])
            self.out_ops.append(op)
        return P.finalize(final_waits=self.out_ops)

    def attn_mla(self, qn_s, qr_s, kn_s, kr_s, v_s, ot_s, ident_b, cmask):
        P, S, NB = self.P, self.S, self.NB
        scale = 1.0 / math.sqrt(192.0)
        qn = [self.lalloc(f"qn{i}", [128, S], BF16, dma=True) for i in range(2)]
        kn = [self.lalloc(f"kn{i}", [128, S], BF16, dma=True) for i in range(2)]
        vv = [self.lalloc(f"vv{i}", [128, NB, 128], BF16, dma=True) for i in range(2)]
        qr = [self.lalloc(f"qr{i}", [128, S], BF16, dma=True) for i in range(2)]
        kr = self.lalloc("kr", [128, S], BF16, dma=True)
        pb = [self.lalloc(f"pb{i}", [128, 512], BF16) for i in range(4)]
        pts = [self.lalloc(f"pts{i}", [128, 4, 128], BF16) for i in range(4)]
        sdg = [self.lalloc(f"sdg{i}", [128, 128], F32) for i in range(2)]
        NQ = 6
        mst = [self.lalloc(f"mst{i}", [128, 16], F32) for i in range(NQ)]
        nbst = [self.lalloc(f"nbst{i}", [128, 16], F32) for i in range(NQ)]
        lst = [self.lalloc(f"lst{i}", [128, 16], F32) for i in range(NQ)]
        wst = [self.lalloc(f"wst{i}", [128, 16], F32) for i in range(NQ)]
        cst = [self.lalloc(f"cst{i}", [128, 4], F32) for i in range(NQ)]
        ocs = [self.lalloc(f"ocs{i}", [128, 9, 128], F32) for i in range(NQ)]
        oacc = [self.lalloc(f"oacc{i}", [128, 128], F32) for i in range(2)]
        on = [self.lalloc(f"on{i}", [128, 128], BF16) for i in range(2)]
        otst = [self.lalloc(f"otst{i}", [128, S], BF16, dma=True) for i in range(2)]
        psS = self.psb[0:4]
        ptp = [self.pst[0], self.pst[1]]
        Oq = [self.psb[4], self.psb[5]]
        P.dma("sp", kr.ap[:, :], kr_s.ap[:, :], reads=[kr_s], writes=[kr])

        def load_head(h):
            P.dma("sp", qn[h % 2].ap[:, :], qn_s.ap[h], reads=[qn_s], writes=[qn[h % 2]])
            P.dma("sp", kn[h % 2].ap[:, :], kn_s.ap[h], reads=[kn_s], writes=[kn[h % 2]])
            P.dma("sp", qr[h % 2].ap[:, :], qr_s.ap[h // 2], reads=[qr_s], writes=[qr[h % 2]])
            P.dma("sp", vv[h % 2].ap[:, :, :], v_s.ap[:, h * 128:(h + 1) * 128].rearrange("(b p) n -> p b n", p=128),
                  reads=[v_s], writes=[vv[h % 2]])

        chunks = []
        qbi = 0
        for h in range(8):
            for i in range(NB):
                cl = []
                for j0 in range(0, i * 128, 512):
                    cl.append(dict(j0=j0, w=min(512, i * 128 - j0), diag=False))
                cl.append(dict(j0=i * 128, w=128, diag=True))
                for ci, c in enumerate(cl):
                    c.update(h=h, i=i, ci=ci, nc=len(cl), last=(ci == len(cl) - 1), qb=qbi, first=(ci == 0))
                    chunks.append(c)
                qbi += 1
        NCH = len(chunks)
        first_of_head = {}
        for n, c in enumerate(chunks):
            first_of_head.setdefault(c["h"], n)
        cnt = {"ev": 0, "tp": 0}
        pend = []

        def stA(n):
            c = chunks[n]
            h, i, j0, w = c["h"], c["i"], c["j0"], c["w"]
            q_, k_, r_ = qn[h % 2], kn[h % 2], qr[h % 2]
            hp = h % 2
            ps = psS[n % 4]
            self.mm(ps.ap[:, :w], q_.ap[:, i * 128:(i + 1) * 128], k_.ap[:, j0:j0 + w], True, False, [q_, k_], [ps])
            self.mm(ps.ap[:, :w], r_.ap[hp * 64:(hp + 1) * 64, i * 128:(i + 1) * 128], kr.ap[hp * 64:(hp + 1) * 64, j0:j0 + w],
                    False, True, [r_, kr], [ps])

        def stB(n):
            c = chunks[n]
            w, ci, q = c["w"], c["ci"], c["qb"] % NQ
            ps = psS[n % 4]
            m_, nb_, l_ = mst[q], nbst[q], lst[q]
            if c["first"]:
                flush(c["qb"] - NQ)
                P.emit("dve", lambda E: E.memset(l_.ap[:, :], 0.0), writes=[l_])
            if c["diag"]:
                sd = sdg[c["qb"] % 2]
                self.tt("dve", sd.ap[:, :], ps.ap[:, 0:128], cmask.ap[:, :], ALU.add, [ps, cmask], [sd])
                src, srcb = sd.ap[:, :], sd
            else:
                src, srcb = ps.ap[:, :w], ps
            P.emit("dve", lambda E: E.tensor_reduce(m_.ap[:, ci:ci + 1], src, AX.X, ALU.max), reads=[srcb], writes=[m_])
            self.ts("dve", nb_.ap[:, ci:ci + 1], m_.ap[:, ci:ci + 1], -scale, None, ALU.mult, None, [m_], [nb_])
            p_ = pb[n % 4]
            self.act(p_.ap[:, :w], src, AF.Exp, [srcb, nb_], [p_, l_], bias=nb_.ap[:, ci:ci + 1], scale=scale, accum=l_.ap[:, ci:ci + 1])
            for _ in range(3):
                if pend:
                    pend.pop(0)[1]()

        def flush(upto_qb):
            while pend and pend[0][0] <= upto_qb:
                pend.pop(0)[1]()

        def stT(n):
            c = chunks[n]
            w = c["w"]
            p_ = pb[n % 4]
            tp_ = ptp[n % 2]
            for j in range(w // 128):
                P.emit("pe", lambda E, j=j, tp_=tp_, p_=p_: E.transpose(tp_.ap[:, j * 128:(j + 1) * 128], p_.ap[:, j * 128:(j + 1) * 128], ident_b.ap[:, :]),
                       reads=[p_, ident_b], writes=[tp_])

        def stD(n):
            c = chunks[n]
            nb = c["w"] // 128
            tp_ = ptp[n % 2]
            pt_ = pts[n % 4]
            self.copy("act", pt_.ap[:, 0:nb, :], tp_.ap[:, 0:nb * 128].rearrange("p (a b) -> p a b", a=nb), [tp_], [pt_])

        def stE(n):
            c = chunks[n]
            h, i, j0, w, ci, q = c["h"], c["i"], c["j0"], c["w"], c["ci"], c["qb"] % NQ
            v_ = vv[h % 2]
            pt_ = pts[n % 4]
            o_ = Oq[n % 2]
            nb = w // 128
            for j in range(nb):
                self.mm(o_.ap[:, 0:128], pt_.ap[:, j, :], v_.ap[:, j0 // 128 + j, :], j == 0, j == nb - 1, [pt_, v_], [o_])
            oc_ = ocs[q]
            self.copy("act", oc_.ap[:, ci, :], o_.ap[:, 0:128], [o_], [oc_])
            if c["last"]:
                combine(n)
            if n == first_of_head[h] and h >= 1 and h + 1 < 8:
                load_head(h + 1)

        def combine(n):
            c = chunks[n]
            h, i, nc, q, qb = c["h"], c["i"], c["nc"], c["qb"] % NQ, c["qb"]
            m_, l_, w_, c_, oc_ = mst[q], lst[q], wst[q], cst[q], ocs[q]
            oa = oacc[qb % 2]
            o_ = on[qb % 2]
            ost = otst[h % 2]
            mo = []
            mo.append(lambda: P.emit("dve", lambda E: E.tensor_reduce(c_.ap[:, 0:1], m_.ap[:, 0:nc], AX.X, ALU.max), reads=[m_], writes=[c_]))
            mo.append(lambda: self.ts("dve", w_.ap[:, 0:nc], m_.ap[:, 0:nc], c_.ap[:, 0:1], None, ALU.subtract, None, [m_, c_], [w_]))
            mo.append(lambda: self.act(w_.ap[:, 0:nc], w_.ap[:, 0:nc], AF.Exp, [w_], [w_], scale=scale))
            mo.append(lambda: self.tt("dve", l_.ap[:, 0:nc], l_.ap[:, 0:nc], w_.ap[:, 0:nc], ALU.mult, [l_, w_], [l_]))
            mo.append(lambda: P.emit("dve", lambda E: E.tensor_reduce(c_.ap[:, 1:2], l_.ap[:, 0:nc], AX.X, ALU.add), reads=[l_], writes=[c_]))
            mo.append(lambda: P.emit("dve", lambda E: E.reciprocal(c_.ap[:, 2:3], c_.ap[:, 1:2]), reads=[c_], writes=[c_]))
            mo.append(lambda: self.ts("dve", w_.ap[:, 0:nc], w_.ap[:, 0:nc], c_.ap[:, 2:3], None, ALU.mult, None, [w_, c_], [w_]))
            for k in range(nc):
                dst = o_ if k == nc - 1 else oa
                if k == 0:
                    mo.append(lambda dst=dst: self.ts("dve", dst.ap[:, :], oc_.ap[:, 0, :], w_.ap[:, 0:1], None, ALU.mult, None, [oc_, w_], [dst]))
                else:
                    mo.append(lambda dst=dst, k=k: self.stt("dve", dst.ap[:, :], oc_.ap[:, k, :], w_.ap[:, k:k + 1], oa.ap[:, :], ALU.mult, ALU.add, [oc_, w_, oa], [dst]))

            def tail():
                tp_ = ptp[cnt["tp"] % 2]
                cnt["tp"] += 1
                P.emit("pe", lambda E: E.transpose(tp_.ap[:, 0:128], o_.ap[:, :], ident_b.ap[:, :]), reads=[o_, ident_b], writes=[tp_])
                self.copy("act", ost.ap[:, i * 128:(i + 1) * 128], tp_.ap[:, 0:128], [tp_], [ost])
                if i == NB - 1:
                    P.dma("act", ot_s.ap[h * 128:(h + 1) * 128, :], ost.ap[:, :], reads=[ost], writes=[ot_s])
            mo.append(tail)
            for f in mo:
                pend.append((qb, f))

        load_head(0)
        load_head(1)
        LA = 4
        for s_ in range(-LA, NCH):
            if 0 <= s_ + LA < NCH:
                stA(s_ + LA)
                stB(s_ + LA)
            if 0 <= s_ + 2 < NCH:
                stT(s_ + 2)
                stD(s_ + 2)
            if 0 <= s_ < NCH:
                stE(s_)
        flush(10 ** 9)

    def attn_sb(self, qsb_s, ksb_s, vsb_s, ot2_s, triU, triL, m01):
        P, S, NB, NT = self.P, self.S, self.NB, self.NT
        scale = 1.0 / math.sqrt(128.0)
        qq = [[self.lalloc(f"qq{c}{i}", [128, S], BF16, dma=True) for i in range(2)] for c in range(2)]
        kk = [[self.lalloc(f"kk{c}{i}", [128, S], BF16, dma=True) for i in range(2)] for c in range(2)]
        vv = [[self.lalloc(f"vv{c}{i}", [128, NB, 128], BF16, dma=True) for i in range(2)] for c in range(2)]
        Eb = [[self.lalloc(f"E{c}{i}", [128, 512], F32) for i in range(4)] for c in range(2)]
        SPb = [[self.lalloc(f"SPb{c}{i}", [128, 512], BF16) for i in range(4)] for c in range(2)]
        Gb = [[self.lalloc(f"G{c}{i}", [128, 512], F32) for i in range(3)] for c in range(2)]
        Ab = [[self.lalloc(f"A{c}{i}", [128, 512], BF16) for i in range(4)] for c in range(2)]
        otst = [self.lalloc(f"otst{c}", [128, S], BF16, dma=True) for c in range(2)]
        Xp = [self.psb[4], self.psb[5]]
        Op = [self.psb[6], self.psb[7]]

        def load_pair(pr):
            for c in range(2):
                h = 2 * pr + c
                P.dma("sp", qq[c][pr % 2].ap[:, :], qsb_s.ap[h], reads=[qsb_s], writes=[qq[c][pr % 2]])
                P.dma("sp", kk[c][pr % 2].ap[:, :], ksb_s.ap[h], reads=[ksb_s], writes=[kk[c][pr % 2]])
                P.dma("sp", vv[c][pr % 2].ap[:, :, :], vsb_s.ap[:, h * 128:(h + 1) * 128].rearrange("(b p) n -> p b n", p=128),
                      reads=[vsb_s], writes=[vv[c][pr % 2]])

        it = 0
        load_pair(0)
        for pr in range(4):
            if pr + 1 < 4:
                load_pair(pr + 1)
            for j in range(NT):
                q0 = j * 512
                kbs = list(range(4 * j + 3, -1, -1))
                nk = len(kbs)
                st = [dict(), dict()]

                zps = [dict(), dict()]

                def zmm(c, n):
                    q_, k_ = qq[c][pr % 2], kk[c][pr % 2]
                    kb = kbs[n]
                    ps = self.next_ps()
                    self.mm(ps.ap[:, :], k_.ap[:, kb * 128:(kb + 1) * 128], q_.ap[:, q0:q0 + 512], True, True, [k_, q_], [ps])
                    zps[c][n] = ps

                def zstage(c, n):
                    kb = kbs[n]
                    o = kb - 4 * j
                    ps = zps[c].pop(n)
                    e_ = Eb[c][(it + n) % 4]
                    self.act(e_.ap[:, :], ps.ap[:, :], AF.Exp, [ps], [e_], scale=scale)
                    if o >= 0:
                        self.tt("dve", e_.ap[:, :], e_.ap[:, :], m01[o].ap[:, :], ALU.mult, [e_, m01[o]], [e_])
                    st[c][n] = (e_,)

                def zstage2(c, n):
                    (e_,) = st[c][n]
                    sb_ = SPb[c][(it + n) % 4]
                    self.act(sb_.ap[:, :], e_.ap[:, :], AF.Ln, [e_], [sb_], bias=1.0)
                    st[c][n] = (e_, sb_)

                def t1(c, n):
                    e_, sb_ = st[c][n]
                    P.emit("pe", lambda E, c=c, n=n, sb_=sb_: E.matmul(Xp[c].ap[:, :], triU.ap[:, :], sb_.ap[:, :], start=(n == 0), stop=True, skip_group_check=True),
                           reads=[triU, sb_], writes=[Xp[c]])

                def t2(c, n):
                    g_ = Gb[c][(it + n) % 3]
                    self.act(g_.ap[:, :], Xp[c].ap[:, :], AF.Exp, [Xp[c]], [g_], scale=-1.0)
                    st[c][n] = st[c][n] + (g_,)

                def t3(c, n):
                    e_, sb_, g_ = st[c][n]
                    if n < nk - 1:
                        P.emit("pe", lambda E, c=c, sb_=sb_: E.matmul(Xp[c].ap[:, :], triL.ap[:, :], sb_.ap[:, :], start=False, stop=True, skip_group_check=True),
                               reads=[triL, sb_], writes=[Xp[c]])

                def t4(c, n):
                    e_, sb_, g_ = st[c][n]
                    a_ = Ab[c][(it + n) % 4]
                    self.tt("dve", a_.ap[:, :], e_.ap[:, :], g_.ap[:, :], ALU.mult, [e_, g_], [a_])
                    st[c][n] = (a_,)

                def pstage(c, n):
                    (a_,) = st[c][n]
                    kb = kbs[n]
                    v_ = vv[c][pr % 2]
                    self.mm(Op[c].ap[:, :], v_.ap[:, kb, :], a_.ap[:, :], n == 0, n == nk - 1, [v_, a_], [Op[c]])

                for c in range(2):
                    zmm(c, 0)
                if nk > 1:
                    for c in range(2):
                        zmm(c, 1)
                for c in range(2):
                    zstage(c, 0)
                for c in range(2):
                    zstage2(c, 0)
                for n in range(nk):
                    if n + 1 < nk:
                        for c in range(2):
                            zstage(c, n + 1)
                        for c in range(2):
                            zstage2(c, n + 1)
                    for c in range(2):
                        t1(c, n)
                    if n + 2 < nk:
                        for c in range(2):
                            zmm(c, n + 2)
                    for c in range(2):
                        t2(c, n)
                    for c in range(2):
                        t3(c, n)
                    for c in range(2):
                        t4(c, n)
                    if n >= 1:
                        for c in range(2):
                            pstage(c, n - 1)
                for c in range(2):
                    pstage(c, nk - 1)
                it += nk
                for c in range(2):
                    self.copy("act", otst[c].ap[:, q0:q0 + 512], Op[c].ap[:, :], [Op[c]], [otst[c]])
            for c in range(2):
                h = 2 * pr + c
                P.dma("act", ot2_s.ap[h * 128:(h + 1) * 128, :], otst[c].ap[:, :], reads=[otst[c]], writes=[ot2_s])


_NC_CACHE = {}


def build_nc(S):
    if S in _NC_CACHE:
        return _NC_CACHE[S]
    nc = bass.Bass("TRN2", target_bir_lowering=False)
    with ExitStack() as es:
        kb = KB(nc, es, S)
        stats = kb.build()
    _NC_CACHE[S] = nc
    return nc


def _pc(v, nch):
    return np.ascontiguousarray(np.asarray(v, np.float32).reshape(nch, 128).T)


def prep_shared(w_mod, b_mod, g_mix, g_ffn, w_a_down, g_q_lat, g_kv_lat, w_uq, w_ukv, w_oa, w_mod_kv, b_mod_kv,
                g_kv, w_kv_sb, w_q_sb, w_o_sb, w_ffn_gu, w_ffn_down, w_router, b_router, w_exp_gu, w_exp_down, g_final):
    f = lambda a: np.ascontiguousarray(np.asarray(a, np.float32))
    sh = {}
    sh["w_mod"] = f(w_mod)
    sh["w_mod_kv"] = f(w_mod_kv)
    sh["bmod"] = np.concatenate([_pc(b_mod[0], 48), _pc(b_mod[1], 48), _pc(b_mod_kv, 16)], axis=1)
    sh["gvec"] = np.concatenate([_pc(g_mix[0], 8), _pc(g_mix[1], 8), _pc(g_ffn[0], 8), _pc(g_ffn[1], 8),
                                 _pc(g_kv, 8), _pc(g_final, 8)], axis=1)
    sh["glat"] = np.concatenate([_pc(g_q_lat[0], 3), _pc(g_kv_lat[0], 2)], axis=1)
    sh["brout"] = f(b_router[0]).reshape(1, NE)
    sh["wrout"] = f(w_router[0])
    p = np.arange(128)
    inv = (10000.0 ** (-(p % 32).astype(np.float64) / 32.0)).astype(np.float32)
    sgn = np.where((p % 64) < 32, -1.0, 1.0).astype(np.float32)
    sh["ropec"] = np.ascontiguousarray(np.stack([inv, sgn], axis=1))
    wad = f(w_a_down[0])
    kr1 = np.arange(640, 672)
    kr2 = np.arange(672, 704)
    colsA = np.concatenate([kr1, kr2, kr1, kr2])
    colsB = np.concatenate([kr2, kr1, kr2, kr1])
    sh["wad"] = np.ascontiguousarray(np.concatenate([wad[:, :640], wad[:, colsA], wad[:, colsB]], axis=1))
    wuq = f(w_uq[0])
    cn = np.concatenate([np.arange(h * 192, h * 192 + 128) for h in range(8)])
    ca, cb = [], []
    for j in range(4):
        for h in (2 * j, 2 * j + 1):
            x1 = np.arange(h * 192 + 128, h * 192 + 160)
            x2 = np.arange(h * 192 + 160, h * 192 + 192)
            ca += [x1, x2]
            cb += [x2, x1]
    sh["wuq"] = np.ascontiguousarray(np.concatenate([wuq[:, cn], wuq[:, np.concatenate(ca)], wuq[:, np.concatenate(cb)]], axis=1))
    wukv = f(w_ukv[0])
    ck = np.concatenate([np.arange(h * 256, h * 256 + 128) for h in range(8)])
    cv = np.concatenate([np.arange(h * 256 + 128, h * 256 + 256) for h in range(8)])
    sh["wukv"] = np.ascontiguousarray(np.concatenate([wukv[:, ck], wukv[:, cv]], axis=1))
    sh["woa"] = f(w_oa[0])
    gu_cols = np.concatenate([np.concatenate([np.arange(c2 * 128, c2 * 128 + 256), DFF + np.arange(c2 * 128, c2 * 128 + 256)])
                              for c2 in range(0, 28, 2)])
    sh["wgu"] = np.ascontiguousarray(f(w_ffn_gu[0])[:, gu_cols])
    sh["wdn"] = f(w_ffn_down[0])
    sh["wkvsb"] = f(w_kv_sb)
    sh["wqsb"] = f(w_q_sb[0])
    sh["wosb"] = f(w_o_sb[0])
    sh["wegu"] = np.ascontiguousarray(f(w_exp_gu[0])[:, :, gu_cols]).reshape(NE * D, 2 * DFF)
    sh["wedn"] = f(w_exp_down[0]).reshape(NE * DFF, D)
    return sh


def run(x, c, positions, sh, S, ncores):
    nc = build_nc(S)
    in_maps = []
    for b in range(ncores):
        m = dict(sh)
        m["xT"] = np.ascontiguousarray(np.asarray(x[b], np.float32).T)
        m["cvec"] = _pc(c[b], 8)
        m["pos"] = np.ascontiguousarray(np.asarray(positions[b], np.int32).reshape(1, S))
        in_maps.append(m)
    res = run_bass_kernel_spmd(nc, in_maps, core_ids=list(range(ncores)))
    out = np.stack([np.ascontiguousarray(res.results[b]["outT"].T) for b in range(ncores)], axis=0)
    return out.astype(np.float32)


def kernel(x, c, positions, w_mod, b_mod, g_mix, g_ffn, w_a_down, g_q_lat, g_kv_lat, w_uq, w_ukv, w_oa,
           w_mod_kv, b_mod_kv, g_kv, w_kv_sb, w_q_sb, w_o_sb, w_ffn_gu, w_ffn_down, w_router, b_router,
           w_exp_gu, w_exp_down, g_final):
    sh = prep_shared(w_mod, b_mod, g_mix, g_ffn, w_a_down, g_q_lat, g_kv_lat, w_uq, w_ukv, w_oa, w_mod_kv, b_mod_kv,
                     g_kv, w_kv_sb, w_q_sb, w_o_sb, w_ffn_gu, w_ffn_down, w_router, b_router, w_exp_gu, w_exp_down, g_final)
    x = np.asarray(x)
    return run(x, np.asarray(c), np.asarray(positions), sh, x.shape[1], x.shape[0])
```

```python
import math
from contextlib import ExitStack
import numpy as np
import concourse.bass as bass
import concourse.mybir as mybir
from concourse.bass_utils import run_bass_kernel_spmd

F32 = mybir.dt.float32
BF16 = mybir.dt.bfloat16
I32 = mybir.dt.int32
AF = mybir.ActivationFunctionType
ALU = mybir.AluOpType
AX = mybir.AxisListType

EPOCH = 30000
D = 1024
DFF = 3584
NE = 8
EPS = 1e-6
TT = 512
SEQ = 4096
NCORES = 8


class Buf:
    __slots__ = ("ap", "name", "w", "r", "dsem")

    def __init__(self, ap=None, name=""):
        self.ap = ap
        self.name = name
        self.w = None
        self.r = {}
        self.dsem = None


class DSem:
    def __init__(self, prog):
        self.prog = prog
        self.sem = prog.new_sem()
        self.count = 0

    def next(self):
        if self.count + 16 > EPOCH:
            self.sem = self.prog.new_sem()
            self.count = 0
        self.count += 16
        return (self.sem, self.count)


class Op:
    __slots__ = ("eng", "fn", "waits", "needed", "idx", "dma", "tok", "dwaits")

    def __init__(self, eng, fn, dma):
        self.eng = eng
        self.fn = fn
        self.waits = []
        self.dwaits = []
        self.needed = False
        self.idx = None
        self.dma = dma
        self.tok = None


class Prog:
    ENGS = ("pe", "act", "dve", "pool", "sp")

    def __init__(self, nc, es):
        self.nc = nc
        self.es = es
        self.q = {e: [] for e in self.ENGS}
        self.seen = {e: {} for e in self.ENGS}
        self.dseen = {e: {} for e in self.ENGS}
        self.nsem = 0
        self.bufs = []
        self.scratch = None
        self.last_barrier = None

    def new_sem(self):
        s = self.es.enter_context(self.nc.semaphore(f"s{self.nsem}"))
        self.nsem += 1
        return s

    def reg(self, b):
        b.w = self.last_barrier
        self.bufs.append(b)
        return b

    def ps(self, name, shape, dt):
        t = self.es.enter_context(self.nc.psum_tensor(name, list(shape), dt))
        return self.reg(Buf(t, name))

    def dram(self, name, shape, dt, kind=None, reg=True):
        if kind is None:
            t = self.nc.dram_tensor(name, list(shape), dt)
        else:
            t = self.nc.dram_tensor(name, list(shape), dt, kind=kind)
        b = Buf(t.ap(), name)
        return self.reg(b) if reg else b

    def _dep(self, op, d):
        if d is None:
            return
        eng = op.eng
        if d.dma:
            sem, val = d.tok
            k = id(sem)
            if self.dseen[eng].get(k, 0) >= val:
                return
            self.dseen[eng][k] = val
            op.dwaits.append((sem, val))
        else:
            if self.seen[eng].get(d.eng, -1) >= d.idx:
                return
            self.seen[eng][d.eng] = d.idx
            d.needed = True
            op.waits.append(d)

    def emit(self, eng, fn, reads=(), writes=(), dma=False, dsem=None):
        op = Op(eng, fn, dma)
        for b in reads:
            w = b.w
            if w is not None and not (w.eng == eng and eng == "pe" and not w.dma and not dma):
                self._dep(op, w)
        for b in writes:
            w = b.w
            if w is not None and not (w.eng == eng and eng == "pe" and not w.dma and not dma):
                self._dep(op, w)
            for r in b.r.values():
                if (not r.dma) and (not dma) and r.eng == eng and eng == "pe":
                    continue
                self._dep(op, r)
        if dma:
            if dsem is None:
                for b in list(writes) + list(reads):
                    if b.dsem is not None:
                        dsem = b.dsem
                        break
            assert dsem is not None
            op.tok = dsem.next()
        op.idx = len(self.q[eng])
        self.q[eng].append(op)
        for b in writes:
            b.w = op
            b.r = {}
        for b in reads:
            key = id(op.tok[0]) if dma else eng
            b.r[key] = op
        return op

    def dma(self, eng, out, in_, reads=(), writes=(), dsem=None):
        return self.emit(eng, lambda E: E.dma_start(out=out, in_=in_),
                         reads=reads, writes=writes, dma=True, dsem=dsem)

    def barrier(self):
        sc = self.scratch
        self.last_barrier = self.emit("pool", lambda E: E.memset(sc.ap[:, 0:1], 0.0), writes=list(self.bufs))

    def finalize(self, final_waits=()):
        nc = self.nc
        for e in self.ENGS:
            k = 0
            sl = []
            for op in self.q[e]:
                if op.dma:
                    continue
                if op.needed:
                    ep, v = divmod(k, EPOCH)
                    while len(sl) <= ep:
                        sl.append(self.new_sem())
                    op.tok = (sl[ep], v + 1)
                    k += 1
        q = self.q

        def run(E, e):
            for op in q[e]:
                for d in op.waits:
                    E.wait_ge(d.tok[0], d.tok[1])
                for (s, v) in op.dwaits:
                    E.wait_ge(s, v)
                ins = op.fn(E)
                if op.dma:
                    ins.then_inc(op.tok[0], 16)
                elif op.needed:
                    ins.then_inc(op.tok[0], 1)

        with nc.Block() as block:
            @block.tensor
            def _(E):
                run(E, "pe")

            @block.scalar
            def _(E):
                run(E, "act")

            @block.vector
            def _(E):
                run(E, "dve")

            @block.gpsimd
            def _(E):
                run(E, "pool")

            @block.sync
            def _(E):
                run(E, "sp")
                for op in final_waits:
                    E.wait_ge(op.tok[0], op.tok[1])
        return {e: (len(q[e]), sum(1 for o in q[e] if o.needed)) for e in self.ENGS}


def _dtsize(dt):
    return 2 if dt == BF16 else 4


class KB:
    ARENA_KB = 200
    CONST_KB = 12

    def __init__(self, nc, es, S):
        self.nc = nc
        self.S = S
        self.NT = S // TT
        self.NB = S // 128
        P = self.P = Prog(nc, es)
        self.arena = es.enter_context(nc.sbuf_tensor("arena", [128, self.ARENA_KB * 256], F32))
        self.coff = 0
        self.loff = self.CONST_KB * 1024
        self.free_dsems = []
        self.local_dsems = []
        P.scratch = self.calloc("scratch", [128, 16], F32)
        self.psb = [P.ps(f"psb{i}", [128, 512], F32) for i in range(8)]
        self.psg = self.psb[0:4]
        self.psa = self.psb[4:6]
        self.pst = [P.reg(Buf(self.psb[6 + i].ap[:, :].bitcast(BF16), f"pst{i}")) for i in range(2)]
        self.psg_i = 0
        self.out_ops = []

    def _view(self, off, shape, dt):
        n = 1
        for s in shape[1:]:
            n *= s
        nb = n * _dtsize(dt)
        assert off % 4 == 0 and nb % 4 == 0
        a = self.arena[:, off // 4: (off + nb) // 4]
        if dt != F32:
            a = a.bitcast(dt)
        if len(shape) == 3:
            a = a.rearrange("p (a b) -> p a b", a=shape[1])
        elif len(shape) == 4:
            a = a.rearrange("p (a b c) -> p a b c", a=shape[1], b=shape[2])
        if shape[0] != 128:
            a = a[0:shape[0]]
        return a, nb

    def calloc(self, name, shape, dt, dma=False):
        a, nb = self._view(self.coff, shape, dt)
        self.coff += (nb + 63) // 64 * 64
        assert self.coff <= self.CONST_KB * 1024, (name, self.coff)
        b = self.P.reg(Buf(a, name))
        if dma:
            b.dsem = DSem(self.P)
        return b

    def lalloc(self, name, shape, dt, dma=False):
        a, nb = self._view(self.loff, shape, dt)
        self.loff += (nb + 63) // 64 * 64
        assert self.loff <= self.ARENA_KB * 1024, (name, self.loff)
        b = self.P.reg(Buf(a, name))
        if dma:
            if self.free_dsems:
                b.dsem = self.free_dsems.pop()
            else:
                b.dsem = DSem(self.P)
            self.local_dsems.append(b.dsem)
        return b

    def phase_end(self):
        self.P.barrier()
        self.loff = self.CONST_KB * 1024
        self.free_dsems.extend(self.local_dsems)
        self.local_dsems = []

    def next_ps(self):
        b = self.psg[self.psg_i % 4]
        self.psg_i += 1
        return b

    def mm(self, out, lhsT, rhs, start, stop, reads, writes):
        self.P.emit("pe", lambda E: E.matmul(out, lhsT, rhs, start=start, stop=stop), reads=reads, writes=writes)

    def act(self, out, in_, func, reads, writes, bias=0.0, scale=1.0, accum=None):
        if accum is None:
            self.P.emit("act", lambda E: E.activation(out, in_, func, bias=bias, scale=scale), reads=reads, writes=writes)
        else:
            self.P.emit("act", lambda E: E.activation(out, in_, func, bias=bias, scale=scale, accum_out=accum), reads=reads, writes=writes)

    def copy(self, eng, out, in_, reads, writes):
        if eng == "act":
            self.P.emit("act", lambda E: E.copy(out, in_), reads=reads, writes=writes)
        else:
            self.P.emit(eng, lambda E: E.tensor_copy(out, in_), reads=reads, writes=writes)

    def tt(self, eng, out, in0, in1, op, reads, writes):
        self.P.emit(eng, lambda E: E.tensor_tensor(out, in0, in1, op), reads=reads, writes=writes)

    def ts(self, eng, out, in0, s1, s2, op0, op1, reads, writes):
        if op1 is None:
            self.P.emit(eng, lambda E: E.tensor_scalar(out, in0, s1, None, op0), reads=reads, writes=writes)
        else:
            self.P.emit(eng, lambda E: E.tensor_scalar(out, in0, s1, s2, op0, op1), reads=reads, writes=writes)

    def stt(self, eng, out, in0, scalar, in1, op0, op1, reads, writes):
        self.P.emit(eng, lambda E: E.scalar_tensor_tensor(out, in0, scalar, in1, op0, op1), reads=reads, writes=writes)

    def wload(self, wd, K, c0, ncols, r0=0):
        KC = K // 128
        wb = self.wbufs[self.wb_i % len(self.wbufs)]
        self.wb_i += 1
        v = wb.ap[:, 0:KC * ncols].rearrange("p (c n) -> p c n", c=KC)
        src = wd.ap[r0:r0 + K, c0:c0 + ncols].rearrange("(c p) n -> p c n", p=128)
        self.P.dma("sp", v, src, reads=[wd], writes=[wb])
        return wb, v

    def lin_fm(self, wd, K, c0, ncols, rhs, rhs_bufs, N, consume, group=512, r0=0):
        KC = K // 128
        for g0 in range(c0, c0 + ncols, group):
            gw = min(group, c0 + ncols - g0)
            wb, v = self.wload(wd, K, g0, gw, r0=r0)
            for j in range(gw // 128):
                ps = self.next_ps()
                for kc in range(KC):
                    self.mm(ps.ap[:, :N], v[:, kc, j * 128:(j + 1) * 128], rhs(kc), kc == 0, kc == KC - 1,
                            [wb] + rhs_bufs, [ps])
                consume((g0 - c0) // 128 + j, ps)

    def lin_tm(self, wd, K, c0, ncols, lhs, lhs_bufs, nblk, consume, group=512):
        KC = K // 128
        for g0 in range(c0, c0 + ncols, group):
            gw = min(group, c0 + ncols - g0)
            wb, v = self.wload(wd, K, g0, gw)
            for blk in range(nblk):
                ps = self.next_ps()
                for kc in range(KC):
                    self.mm(ps.ap[:, :gw], lhs(kc, blk), v[:, kc, :], kc == 0, kc == KC - 1, [wb] + lhs_bufs, [ps])
                consume(blk, g0 - c0, gw, ps)

    def norm_mod(self, x, nch, nfeat, scale_ap, bias_ap, outs):
        P = self.P
        ps = self.next_ps()
        for c in range(nch):
            sq = self.tmp()
            self.act(sq.ap[:, :], x.ap[:, c, :], AF.Square, [x], [sq])
            self.mm(ps.ap[:, :], self.ones_f.ap[:, :], sq.ap[:, :], c == 0, c == nch - 1, [self.ones_f, sq], [ps])
        rstd = self.rstd_t[self.rstd_i % 2]
        self.rstd_i += 1
        self.ts("dve", rstd.ap[:, :], ps.ap[:, :], 1.0 / nfeat, EPS, ALU.mult, ALU.add, [ps], [rstd])
        self.act(rstd.ap[:, :], rstd.ap[:, :], AF.Sqrt, [rstd], [rstd])
        P.emit("dve", lambda E: E.reciprocal(rstd.ap[:, :], rstd.ap[:, :]), reads=[rstd], writes=[rstd])
        for c in range(nch):
            t = self.tmp()
            self.tt("dve", t.ap[:, :], x.ap[:, c, :], rstd.ap[:, :], ALU.mult, [x, rstd], [t])
            for (o, sbufs) in outs:
                b = bias_ap(c) if bias_ap is not None else 0.0
                self.act(o.ap[:, c, :], t.ap[:, :], AF.Identity, [t] + sbufs, [o], bias=b, scale=scale_ap(c))

    def tmp(self):
        t = self.tmps[self.tmp_i % len(self.tmps)]
        self.tmp_i += 1
        return t

    def build(self):
        P, S, NT, NB = self.P, self.S, self.NT, self.NB
        nc = self.nc
        xT = P.dram("xT", [D, S], F32, kind="ExternalInput")
        cvec = P.dram("cvec", [128, 8], F32, kind="ExternalInput")
        pos = P.dram("pos", [1, S], I32, kind="ExternalInput")
        ropec = P.dram("ropec", [128, 2], F32, kind="ExternalInput")
        w_mod = P.dram("w_mod", [2, D, 6 * D], F32, kind="ExternalInput")
        w_mod_kv = P.dram("w_mod_kv", [D, 2 * D], F32, kind="ExternalInput")
        bmod = P.dram("bmod", [128, 112], F32, kind="ExternalInput")
        gvec = P.dram("gvec", [128, 48], F32, kind="ExternalInput")
        glat = P.dram("glat", [128, 5], F32, kind="ExternalInput")
        brout = P.dram("brout", [1, NE], F32, kind="ExternalInput")
        wrout = P.dram("wrout", [D, NE], F32, kind="ExternalInput")
        wf = {}
        wshape = {"wad": [D, 896], "wuq": [384, 2048], "wukv": [256, 2048], "woa": [D, D],
                  "wgu": [D, 2 * DFF], "wdn": [DFF, D], "wkvsb": [D, 2 * D], "wqsb": [D, D], "wosb": [D, D],
                  "wegu": [NE * D, 2 * DFF], "wedn": [NE * DFF, D]}
        worder = ["wad", "wuq", "wukv", "woa", "wgu", "wdn", "wkvsb", "wqsb", "wosb", "wegu", "wedn"]
        for n in worder:
            wf[n] = P.dram(n, wshape[n], F32, kind="ExternalInput", reg=False)
        outT = P.dram("outT", [D, S], F32, kind="ExternalOutput")
        wb16 = {n: P.dram(n + "_b", wshape[n], BF16, reg=False) for n in worder}
        for n in worder:
            wb16[n].dsem = DSem(P)
        qn_s = P.dram("qn_s", [8, 128, S], BF16)
        qr_s = P.dram("qr_s", [4, 128, S], BF16)
        kn_s = P.dram("kn_s", [8, 128, S], BF16)
        kr_s = P.dram("kr_s", [128, S], BF16)
        v_s = P.dram("v_s", [S, D], BF16)
        ot_s = P.dram("ot_s", [D, S], BF16)
        x2_s = P.dram("x2_s", [D, S], F32)
        ksb_s = P.dram("ksb_s", [8, 128, S], BF16)
        vsb_s = P.dram("vsb_s", [S, D], BF16)
        qsb_s = P.dram("qsb_s", [8, 128, S], BF16)
        ot2_s = P.dram("ot2_s", [D, S], BF16)

        def cast_weights(names):
            for n in names:
                rows = wshape[n][0]
                step = 128
                for r in range(0, rows, step):
                    P.dma("pool", wb16[n].ap[r:r + step, :], wf[n].ap[r:r + step, :], reads=[wf[n]], writes=[wb16[n]],
                          dsem=wb16[n].dsem)

        ident_f = self.calloc("ident_f", [128, 128], F32)
        self.ones_f = self.calloc("ones_f", [128, 128], F32)
        ident_b = self.calloc("ident_b", [128, 128], BF16)
        triU = self.calloc("triU", [128, 128], BF16)
        triL = self.calloc("triL", [128, 128], BF16)
        cmask = self.calloc("cmask", [128, 128], F32)
        m01 = [self.calloc(f"m01_{o}", [128, 512], BF16) for o in range(4)]
        silc = self.calloc("silc", [128, 8], F32, dma=True)
        modv = self.calloc("modv", [128, 112], F32)
        bmod_t = self.calloc("bmod_t", [128, 112], F32, dma=True)
        gvec_t = self.calloc("gvec_t", [128, 48], F32, dma=True)
        glat_t = self.calloc("glat_t", [128, 5], F32, dma=True)
        ropec_t = self.calloc("ropec_t", [128, 2], F32, dma=True)
        brout_t = self.calloc("brout_t", [128, NE], F32, dma=True)
        wrout_t = self.calloc("wrout_t", [128, 8, NE], F32, dma=True)
        avec = self.calloc("avec", [128, 5, 8], F32)
        tmpc = self.calloc("tmpc", [128, 128], F32)

        def cset(buf, pattern, cm, base, cmp, fill, val=1.0):
            P.emit("pool", lambda E: E.memset(tmpc.ap[:, :], val), writes=[tmpc])
            P.emit("pool", lambda E: E.affine_select(tmpc.ap[:, :], tmpc.ap[:, :], pattern, cmp, fill, base=base,
                                                     channel_multiplier=cm), reads=[tmpc], writes=[tmpc])
            self.copy("pool", buf.ap[:, :], tmpc.ap[:, :], [tmpc], [buf])

        cset(ident_f, [[-1, 128]], 1, 0, ALU.is_equal, 0.0)
        cset(ident_b, [[-1, 128]], 1, 0, ALU.is_equal, 0.0)
        cset(triU, [[-1, 128]], 1, 0, ALU.is_ge, 0.0)
        cset(triL, [[1, 128]], -1, 0, ALU.is_gt, 0.0)
        cset(cmask, [[-1, 128]], 1, 0, ALU.is_ge, -1.0e9, val=0.0)
        P.emit("pool", lambda E: E.memset(self.ones_f.ap[:, :], 1.0), writes=[self.ones_f])

        cast_weights(["wad", "wuq", "wukv"])
        P.dma("sp", silc.ap[:, :], cvec.ap[:, :], reads=[cvec], writes=[silc])
        P.dma("sp", bmod_t.ap[:, :], bmod.ap[:, :], reads=[bmod], writes=[bmod_t])
        P.dma("sp", gvec_t.ap[:, :], gvec.ap[:, :], reads=[gvec], writes=[gvec_t])
        P.dma("sp", glat_t.ap[:, :], glat.ap[:, :], reads=[glat], writes=[glat_t])
        P.dma("sp", ropec_t.ap[:, :], ropec.ap[:, :], reads=[ropec], writes=[ropec_t])
        P.dma("sp", brout_t.ap[:, :], brout.ap[0:1, :].partition_broadcast(128), reads=[brout], writes=[brout_t])
        P.dma("sp", wrout_t.ap[:, :, :], wrout.ap.rearrange("(c p) e -> p c e", p=128), reads=[wrout], writes=[wrout_t])
        self.act(silc.ap[:, :], silc.ap[:, :], AF.Silu, [silc], [silc])

        cosT = self.lalloc("cosT", [128, S], F32)
        sinT = self.lalloc("sinT", [128, S], F32)
        m01f = self.lalloc("m01f", [128, 512], F32)
        for o in range(4):
            P.emit("pool", lambda E: E.memset(m01f.ap[:, :], 1.0), writes=[m01f])
            P.emit("pool", lambda E, o=o: E.affine_select(m01f.ap[:, :], m01f.ap[:, :], [[1, 512]], ALU.is_gt, 0.0,
                                                          base=-128 * o, channel_multiplier=-1), reads=[m01f], writes=[m01f])
            self.copy("pool", m01[o].ap[:, :], m01f.ap[:, :], [m01f], [m01[o]])
        wmb = [self.lalloc(f"wmb{i}", [128, 8, 512], F32, dma=True) for i in range(2)]
        psm = self.psa[0]
        gi = 0
        for (wsrc, ncol, cbase) in ((w_mod.ap[0], 6 * D, 0), (w_mod.ap[1], 6 * D, 48), (w_mod_kv.ap, 2 * D, 96)):
            for g0 in range(0, ncol, 512):
                wb = wmb[gi % 2]
                gi += 1
                P.dma("sp", wb.ap[:, :, :], wsrc[:, g0:g0 + 512].rearrange("(c p) n -> p c n", p=128),
                      reads=[w_mod, w_mod_kv], writes=[wb])
                for j in range(4):
                    col = cbase + g0 // 128 + j
                    for kc in range(8):
                        self.mm(psm.ap[:, col:col + 1], wb.ap[:, kc, j * 128:(j + 1) * 128], silc.ap[:, kc:kc + 1],
                                kc == 0, kc == 7, [wb, silc], [psm])
        self.tt("dve", modv.ap[:, :], psm.ap[:, 0:112], bmod_t.ap[:, :], ALU.add, [psm, bmod_t], [modv])

        def mk_a(slot, gcol, sccol):
            self.stt("dve", avec.ap[:, slot, :], modv.ap[:, sccol:sccol + 8], 1.0, gvec_t.ap[:, gcol:gcol + 8],
                     ALU.add, ALU.mult, [modv, gvec_t], [avec])
        mk_a(0, 0, 8)
        mk_a(1, 16, 32)
        mk_a(2, 8, 48 + 8)
        mk_a(3, 24, 48 + 32)
        mk_a(4, 32, 104)

        TWO_PI = float(2 * np.pi)
        PI = float(np.pi)
        posi = self.lalloc("posi", [128, S], I32, dma=True)
        ang = self.lalloc("ang", [128, S], F32)
        pt = self.lalloc("pt", [128, S], F32)
        pti = self.lalloc("pti", [128, S], I32)
        P.dma("sp", posi.ap[:, :], pos.ap[0:1, :].partition_broadcast(128), reads=[pos], writes=[posi])
        self.copy("dve", ang.ap[:, :], posi.ap[:, :], [posi], [ang])
        self.ts("dve", ang.ap[:, :], ang.ap[:, :], ropec_t.ap[:, 0:1], None, ALU.mult, None, [ang, ropec_t], [ang])
        for (dst, shift) in ((sinT, 0.0), (cosT, PI / 2)):
            r = dst
            self.ts("dve", r.ap[:, :], ang.ap[:, :], shift, None, ALU.add, None, [ang], [r])
            self.ts("dve", pt.ap[:, :], r.ap[:, :], 1.0 / TWO_PI, 0.5, ALU.mult, ALU.add, [r], [pt])
            self.copy("dve", pti.ap[:, :], pt.ap[:, :], [pt], [pti])
            self.copy("dve", pt.ap[:, :], pti.ap[:, :], [pti], [pt])
            self.stt("dve", r.ap[:, :], pt.ap[:, :], -TWO_PI, r.ap[:, :], ALU.mult, ALU.add, [pt, r], [r])
            self.ts("dve", pt.ap[:, :], r.ap[:, :], PI, -TWO_PI, ALU.is_gt, ALU.mult, [r], [pt])
            self.tt("dve", r.ap[:, :], r.ap[:, :], pt.ap[:, :], ALU.add, [r, pt], [r])
            self.ts("dve", pt.ap[:, :], r.ap[:, :], -PI, TWO_PI, ALU.is_lt, ALU.mult, [r], [pt])
            self.tt("dve", r.ap[:, :], r.ap[:, :], pt.ap[:, :], ALU.add, [r, pt], [r])
            self.ts("dve", r.ap[:, :], r.ap[:, :], PI, -PI, ALU.min, ALU.max, [r], [r])
            self.act(r.ap[:, :], r.ap[:, :], AF.Sin, [r], [r])
        self.ts("dve", sinT.ap[:, :], sinT.ap[:, :], ropec_t.ap[:, 1:2], None, ALU.mult, None, [sinT, ropec_t], [sinT])
        P.barrier()
        self.loff = self.CONST_KB * 1024 + 2 * S * 4
        self.free_dsems.extend(self.local_dsems)
        self.local_dsems = []
        cast_weights(["woa", "wgu", "wdn", "wkvsb", "wqsb", "wosb"])

        def tile_bufs(nw=3):
            self.wbufs = [self.lalloc(f"wb{i}", [128, 7168], BF16, dma=True) for i in range(nw)]
            self.wb_i = 0
            self.tmps = [self.lalloc(f"tmp{i}", [128, 512], F32) for i in range(3)]
            self.tmp_i = 0
            self.rstd_t = [self.lalloc(f"rstd{i}", [128, 512], F32) for i in range(2)]
            self.rstd_i = 0

        tile_bufs()
        xf = [self.lalloc(f"xf{i}", [128, 8, TT], F32, dma=True) for i in range(2)]
        hb = self.lalloc("hb", [128, 8, TT], BF16)
        latq = self.lalloc("latq", [128, 3, TT], F32)
        latkv = self.lalloc("latkv", [128, 2, TT], F32)
        ropeA = self.lalloc("ropeA", [128, TT], F32)
        ropeB = self.lalloc("ropeB", [128, TT], F32)
        cq = self.lalloc("cq", [128, 3, TT], BF16)
        ckv = self.lalloc("ckv", [128, 2, TT], BF16)
        krst = self.lalloc("krst", [128, TT], BF16, dma=True)
        qnst = self.lalloc("qnst", [128, 8, TT], BF16, dma=True)
        qrst = self.lalloc("qrst", [128, 4, TT], BF16, dma=True)
        knst = self.lalloc("knst", [128, 8, TT], BF16, dma=True)
        vst = self.lalloc("vst", [128, 4, D], BF16, dma=True)
        qA = self.lalloc("qA", [128, 4, TT], F32)

        def rope_combine(dst_ap, A_ap, B_ap, t0, reads, dst_buf):
            t1 = self.tmp()
            self.tt("dve", t1.ap[:, :], A_ap, cosT.ap[:, t0:t0 + TT], ALU.mult, reads + [cosT], [t1])
            t2 = self.tmp()
            self.tt("dve", t2.ap[:, :], B_ap, sinT.ap[:, t0:t0 + TT], ALU.mult, reads + [sinT], [t2])
            self.tt("dve", dst_ap, t1.ap[:, :], t2.ap[:, :], ALU.add, [t1, t2], [dst_buf])

        xTv = xT.ap.rearrange("(c p) s -> p c s", p=128)
        for t in range(NT):
            t0 = t * TT
            x = xf[t % 2]
            P.dma("sp", x.ap[:, :, :], xTv[:, :, t0:t0 + TT], reads=[xT], writes=[x])
            self.norm_mod(x, 8, D, lambda c: avec.ap[:, 0, c:c + 1], lambda c: modv.ap[:, c:c + 1],
                          [(hb, [avec, modv])])

            def lat_consume(oc, ps, t0=t0):
                if oc < 3:
                    self.copy("act", latq.ap[:, oc, :], ps.ap[:, :], [ps], [latq])
                elif oc < 5:
                    self.copy("act", latkv.ap[:, oc - 3, :], ps.ap[:, :], [ps], [latkv])
                elif oc == 5:
                    self.copy("act", ropeA.ap[:, :], ps.ap[:, :], [ps], [ropeA])
                else:
                    self.copy("act", ropeB.ap[:, :], ps.ap[:, :], [ps], [ropeB])
            self.lin_fm(wb16["wad"], D, 0, 896, lambda kc: hb.ap[:, kc, :], [hb], TT, lat_consume)
            rope_combine(krst.ap[:, :], ropeA.ap[:, :], ropeB.ap[:, :], t0, [ropeA, ropeB], krst)
            P.dma("act", kr_s.ap[:, t0:t0 + TT], krst.ap[:, :], reads=[krst], writes=[kr_s])
            self.norm_mod(latq, 3, 384, lambda c: glat_t.ap[:, c:c + 1], None, [(cq, [glat_t])])
            self.norm_mod(latkv, 2, 256, lambda c: glat_t.ap[:, 3 + c:4 + c], None, [(ckv, [glat_t])])

            def q_consume(oc, ps, t0=t0):
                if oc < 8:
                    self.copy("act", qnst.ap[:, oc, :], ps.ap[:, :], [ps], [qnst])
                elif oc < 12:
                    self.copy("act", qA.ap[:, oc - 8, :], ps.ap[:, :], [ps], [qA])
                else:
                    j = oc - 12
                    t1 = self.tmp()
                    self.tt("dve", t1.ap[:, :], qA.ap[:, j, :], cosT.ap[:, t0:t0 + TT], ALU.mult, [qA, cosT], [t1])
                    t2 = self.tmp()
                    self.tt("dve", t2.ap[:, :], ps.ap[:, :], sinT.ap[:, t0:t0 + TT], ALU.mult, [ps, sinT], [t2])
                    self.tt("dve", qrst.ap[:, j, :], t1.ap[:, :], t2.ap[:, :], ALU.add, [t1, t2], [qrst])
            self.lin_fm(wb16["wuq"], 384, 0, 2048, lambda kc: cq.ap[:, kc, :], [cq], TT, q_consume)
            P.dma("act", qn_s.ap[:, :, t0:t0 + TT].rearrange("h p s -> p h s"), qnst.ap[:, :, :], reads=[qnst], writes=[qn_s])
            P.dma("act", qr_s.ap[:, :, t0:t0 + TT].rearrange("h p s -> p h s"), qrst.ap[:, :, :], reads=[qrst], writes=[qr_s])

            def kn_consume(oc, ps):
                self.copy("act", knst.ap[:, oc, :], ps.ap[:, :], [ps], [knst])
            self.lin_fm(wb16["wukv"], 256, 0, 1024, lambda kc: ckv.ap[:, kc, :], [ckv], TT, kn_consume)
            P.dma("act", kn_s.ap[:, :, t0:t0 + TT].rearrange("h p s -> p h s"), knst.ap[:, :, :], reads=[knst], writes=[kn_s])

            def v_consume(blk, c0, gw, ps):
                self.copy("act", vst.ap[:, blk, c0:c0 + gw], ps.ap[:, :gw], [ps], [vst])
            self.lin_tm(wb16["wukv"], 256, 1024, 1024, lambda kc, blk: ckv.ap[:, kc, blk * 128:(blk + 1) * 128], [ckv], 4, v_consume)
            P.dma("act", v_s.ap[t0:t0 + TT, :].rearrange("(b p) n -> p b n", p=128), vst.ap[:, :, :], reads=[vst], writes=[v_s])
        self.phase_end()
        cast_weights(["wegu", "wedn"])

        self.attn_mla(qn_s, qr_s, kn_s, kr_s, v_s, ot_s, ident_b, cmask)
        self.phase_end()

        tile_bufs()
        xf = [self.lalloc(f"xf{i}", [128, 8, TT], F32, dma=True) for i in range(2)]
        otl = [self.lalloc(f"otl{i}", [128, 8, TT], BF16, dma=True) for i in range(2)]
        hb = self.lalloc("hb", [128, 8, TT], BF16)
        actb = self.lalloc("actb", [128, 28, TT], BF16)
        kst = self.lalloc("kst", [128, 8, TT], BF16, dma=True)
        vst = self.lalloc("vst", [128, 4, D], BF16, dma=True)
        qst = self.lalloc("qst", [128, 8, TT], BF16, dma=True)
        otv = ot_s.ap.rearrange("(c p) s -> p c s", p=128)
        x2v = x2_s.ap.rearrange("(c p) s -> p c s", p=128)

        def resid_consume(x, gcol):
            def f(oc, ps):
                self.stt("dve", x.ap[:, oc, :], ps.ap[:, :], modv.ap[:, gcol + oc:gcol + oc + 1], x.ap[:, oc, :],
                         ALU.mult, ALU.add, [ps, modv, x], [x])
            return f

        def ffn(wgu_d, wdn_d, r0gu, r0dn, hb, down_consume, comb=None):
            for c2 in range(0, 28, 2):
                wb = self.wbufs[self.wb_i % len(self.wbufs)]
                self.wb_i += 1
                v = wb.ap[:, 0:8 * 512].rearrange("p (c n) -> p c n", c=8)
                P.dma("sp", v, wgu_d.ap[r0gu:r0gu + D, (c2 // 2) * 512:(c2 // 2) * 512 + 512].rearrange("(c p) n -> p c n", p=128),
                      reads=[wgu_d], writes=[wb])
                for j in range(2):
                    pg = self.next_ps()
                    for kc in range(8):
                        self.mm(pg.ap[:, :], v[:, kc, j * 128:(j + 1) * 128], hb.ap[:, kc, :], kc == 0, kc == 7, [wb, hb], [pg])
                    pu = self.next_ps()
                    for kc in range(8):
                        self.mm(pu.ap[:, :], v[:, kc, 256 + j * 128:256 + (j + 1) * 128], hb.ap[:, kc, :], kc == 0, kc == 7, [wb, hb], [pu])
                    sg = self.tmp()
                    self.act(sg.ap[:, :], pg.ap[:, :], AF.Silu, [pg], [sg])
                    if comb is None:
                        self.tt("dve", actb.ap[:, c2 + j, :], sg.ap[:, :], pu.ap[:, :], ALU.mult, [sg, pu], [actb])
                    else:
                        s2 = self.tmp()
                        self.tt("dve", s2.ap[:, :], sg.ap[:, :], pu.ap[:, :], ALU.mult, [sg, pu], [s2])
                        self.tt("dve", actb.ap[:, c2 + j, :], s2.ap[:, :], comb[0], ALU.mult, [s2, comb[1]], [actb])
            self.lin_fm(wdn_d, DFF, 0, D, lambda kc: actb.ap[:, kc, :], [actb], TT, down_consume, group=256, r0=r0dn)

        for t in range(NT):
            t0 = t * TT
            x = xf[t % 2]
            ot = otl[t % 2]
            P.dma("sp", x.ap[:, :, :], xTv[:, :, t0:t0 + TT], reads=[xT], writes=[x])
            P.dma("sp", ot.ap[:, :, :], otv[:, :, t0:t0 + TT], reads=[ot_s], writes=[ot])
            self.lin_fm(wb16["woa"], D, 0, D, lambda kc: ot.ap[:, kc, :], [ot], TT, resid_consume(x, 16))
            self.norm_mod(x, 8, D, lambda c: avec.ap[:, 1, c:c + 1], lambda c: modv.ap[:, 24 + c:25 + c], [(hb, [avec, modv])])
            ffn(wb16["wgu"], wb16["wdn"], 0, 0, hb, resid_consume(x, 40))
            P.dma("act", x2v[:, :, t0:t0 + TT], x.ap[:, :, :], reads=[x], writes=[x2_s])
            self.norm_mod(x, 8, D, lambda c: avec.ap[:, 4, c:c + 1], lambda c: modv.ap[:, 96 + c:97 + c], [(hb, [avec, modv])])

            def k_consume(oc, ps):
                self.copy("act", kst.ap[:, oc, :], ps.ap[:, :], [ps], [kst])
            self.lin_fm(wb16["wkvsb"], D, 0, D, lambda kc: hb.ap[:, kc, :], [hb], TT, k_consume)
            P.dma("act", ksb_s.ap[:, :, t0:t0 + TT].rearrange("h p s -> p h s"), kst.ap[:, :, :], reads=[kst], writes=[ksb_s])

            def v_consume(blk, c0, gw, ps):
                self.copy("act", vst.ap[:, blk, c0:c0 + gw], ps.ap[:, :gw], [ps], [vst])
            self.lin_tm(wb16["wkvsb"], D, D, D, lambda kc, blk: hb.ap[:, kc, blk * 128:(blk + 1) * 128], [hb], 4, v_consume)
            P.dma("act", vsb_s.ap[t0:t0 + TT, :].rearrange("(b p) n -> p b n", p=128), vst.ap[:, :, :], reads=[vst], writes=[vsb_s])
            self.norm_mod(x, 8, D, lambda c: avec.ap[:, 2, c:c + 1], lambda c: modv.ap[:, 48 + c:49 + c], [(hb, [avec, modv])])

            def q_consume(oc, ps):
                self.copy("act", qst.ap[:, oc, :], ps.ap[:, :], [ps], [qst])
            self.lin_fm(wb16["wqsb"], D, 0, D, lambda kc: hb.ap[:, kc, :], [hb], TT, q_consume)
            P.dma("act", qsb_s.ap[:, :, t0:t0 + TT].rearrange("h p s -> p h s"), qst.ap[:, :, :], reads=[qst], writes=[qsb_s])
        self.phase_end()

        self.attn_sb(qsb_s, ksb_s, vsb_s, ot2_s, triU, triL, m01)
        self.phase_end()

        tile_bufs()
        xf = [self.lalloc(f"xf{i}", [128, 8, TT], F32, dma=True) for i in range(2)]
        otl = [self.lalloc("otl0", [128, 8, TT], BF16, dma=True)] * 2
        hb = self.lalloc("hb", [128, 8, TT], BF16)
        hf = self.lalloc("hf", [128, 8, TT], F32, dma=True)
        actb = self.lalloc("actb", [128, 28, TT], BF16)
        yacc = self.lalloc("yacc", [128, 8, TT], F32)
        combB = self.lalloc("combB", [128, NE, TT], F32)
        outst = hf
        lg = self.lalloc("lg", [128, 4, NE], F32)
        rt = [self.lalloc(f"rt{i}", [128, 4, NE], F32) for i in range(4)]
        rs = [self.lalloc(f"rs{i}", [128, 4], F32) for i in range(6)]
        dg = [self.lalloc(f"dg{i}", [128, 128], F32) for i in range(3)]
        ot2v = ot2_s.ap.rearrange("(c p) s -> p c s", p=128)
        outv = outT.ap.rearrange("(c p) s -> p c s", p=128)
        dgi = 0
        for t in range(NT):
            t0 = t * TT
            x = xf[t % 2]
            ot = otl[t % 2]
            P.dma("sp", x.ap[:, :, :], x2v[:, :, t0:t0 + TT], reads=[x2_s], writes=[x])
            P.dma("sp", ot.ap[:, :, :], ot2v[:, :, t0:t0 + TT], reads=[ot2_s], writes=[ot])
            self.lin_fm(wb16["wosb"], D, 0, D, lambda kc: ot.ap[:, kc, :], [ot], TT, resid_consume(x, 48 + 16))
            self.norm_mod(x, 8, D, lambda c: avec.ap[:, 3, c:c + 1], lambda c: modv.ap[:, 48 + 24 + c:48 + 25 + c],
                          [(hb, [avec, modv]), (hf, [avec, modv])])
            for blk in range(4):
                ps = self.next_ps()
                for kc in range(8):
                    self.mm(ps.ap[:, 0:NE], hf.ap[:, kc, blk * 128:(blk + 1) * 128], wrout_t.ap[:, kc, :], kc == 0, kc == 7,
                            [hf, wrout_t], [ps])
                self.tt("dve", lg.ap[:, blk, :], ps.ap[:, 0:NE], brout_t.ap[:, :], ALU.add, [ps, brout_t], [lg])
            m1, m2, dd, ee, w1, w2 = rs
            eq1, lg2, eq2, comb = rt
            P.emit("dve", lambda E: E.tensor_reduce(m1.ap[:, :], lg.ap[:, :, :], AX.X, ALU.max), reads=[lg], writes=[m1])
            self.tt("dve", eq1.ap[:, :, :], lg.ap[:, :, :], m1.ap[:, :].unsqueeze(2).to_broadcast([128, 4, NE]), ALU.is_equal, [lg, m1], [eq1])
            self.stt("dve", lg2.ap[:, :, :], eq1.ap[:, :, :], -1.0e30, lg.ap[:, :, :], ALU.mult, ALU.add, [eq1, lg], [lg2])
            P.emit("dve", lambda E: E.tensor_reduce(m2.ap[:, :], lg2.ap[:, :, :], AX.X, ALU.max), reads=[lg2], writes=[m2])
            self.tt("dve", eq2.ap[:, :, :], lg2.ap[:, :, :], m2.ap[:, :].unsqueeze(2).to_broadcast([128, 4, NE]), ALU.is_equal, [lg2, m2], [eq2])
            self.tt("dve", dd.ap[:, :], m2.ap[:, :], m1.ap[:, :], ALU.subtract, [m1, m2], [dd])
            self.act(ee.ap[:, :], dd.ap[:, :], AF.Exp, [dd], [ee])
            self.ts("dve", w1.ap[:, :], ee.ap[:, :], 1.0, None, ALU.add, None, [ee], [w1])
            P.emit("dve", lambda E: E.reciprocal(w1.ap[:, :], w1.ap[:, :]), reads=[w1], writes=[w1])
            self.tt("dve", w2.ap[:, :], ee.ap[:, :], w1.ap[:, :], ALU.mult, [ee, w1], [w2])
            self.tt("dve", eq1.ap[:, :, :], eq1.ap[:, :, :], w1.ap[:, :].unsqueeze(2).to_broadcast([128, 4, NE]), ALU.mult, [eq1, w1], [eq1])
            self.tt("dve", eq2.ap[:, :, :], eq2.ap[:, :, :], w2.ap[:, :].unsqueeze(2).to_broadcast([128, 4, NE]), ALU.mult, [eq2, w2], [eq2])
            self.tt("dve", comb.ap[:, :, :], eq1.ap[:, :, :], eq2.ap[:, :, :], ALU.add, [eq1, eq2], [comb])
            for e in range(NE):
                ps = self.next_ps()
                for blk in range(4):
                    d = dg[dgi % 3]
                    dgi += 1
                    self.ts("dve", d.ap[:, :], ident_f.ap[:, :], comb.ap[:, blk, e:e + 1], None, ALU.mult, None, [ident_f, comb], [d])
                    self.mm(ps.ap[:, blk * 128:(blk + 1) * 128], self.ones_f.ap[:, :], d.ap[:, :], True, True, [self.ones_f, d], [ps])
                self.copy("act", combB.ap[:, e, :], ps.ap[:, :], [ps], [combB])
            for e in range(NE):
                def y_consume(oc, ps, e=e):
                    if e == 0:
                        self.copy("dve", yacc.ap[:, oc, :], ps.ap[:, :], [ps], [yacc])
                    else:
                        self.tt("dve", yacc.ap[:, oc, :], yacc.ap[:, oc, :], ps.ap[:, :], ALU.add, [ps, yacc], [yacc])
                ffn(wb16["wegu"], wb16["wedn"], e * D, e * DFF, hb, y_consume, comb=(combB.ap[:, e, :], combB))
            for oc in range(8):
                self.stt("dve", x.ap[:, oc, :], yacc.ap[:, oc, :], modv.ap[:, 48 + 40 + oc:48 + 41 + oc], x.ap[:, oc, :],
                         ALU.mult, ALU.add, [yacc, modv, x], [x])
            self.norm_mod(x, 8, D, lambda c: gvec_t.ap[:, 40 + c:41 + c], None, [(outst, [gvec_t])])
            op = P.dma("act", outv[:, :, t0:t0 + TT], outst.ap[:, :, :], reads=[outst], writes=[outT])
            self.out_ops.append(op)
        return P.finalize(final_waits=self.out_ops)

    def attn_mla(self, qn_s, qr_s, kn_s, kr_s, v_s, ot_s, ident_b, cmask):
        P, S, NB = self.P, self.S, self.NB
        scale = 1.0 / math.sqrt(192.0)
        qn = [self.lalloc(f"qn{i}", [128, S], BF16, dma=True) for i in range(2)]
        kn = [self.lalloc(f"kn{i}", [128, S], BF16, dma=True) for i in range(2)]
        vv = [self.lalloc(f"vv{i}", [128, NB, 128], BF16, dma=True) for i in range(2)]
        qr = [self.lalloc(f"qr{i}", [128, S], BF16, dma=True) for i in range(2)]
        kr = self.lalloc("kr", [128, S], BF16, dma=True)
        pb = [self.lalloc(f"pb{i}", [128, 512], BF16) for i in range(4)]
        pts = [self.lalloc(f"pts{i}", [128, 4, 128], BF16) for i in range(4)]
        sdg = [self.lalloc(f"sdg{i}", [128, 128], F32) for i in range(2)]
        NQ = 6
        mst = [self.lalloc(f"mst{i}", [128, 16], F32) for i in range(NQ)]
        nbr = [self.lalloc(f"nbr{i}", [128, 1], F32) for i in range(8)]
        lst = [self.lalloc(f"lst{i}", [128, 16], F32) for i in range(NQ)]
        wst = [self.lalloc(f"wst{i}", [128, 16], F32) for i in range(NQ)]
        cst = [self.lalloc(f"cst{i}", [128, 4], F32) for i in range(NQ)]
        ocs = [self.lalloc(f"ocs{i}", [128, 9, 128], F32) for i in range(NQ)]
        oacc = [self.lalloc(f"oacc{i}", [128, 128], F32) for i in range(2)]
        on = [self.lalloc(f"on{i}", [128, 128], BF16) for i in range(2)]
        otst = [self.lalloc(f"otst{i}", [128, S], BF16, dma=True) for i in range(2)]
        psS = self.psb[0:4]
        ptp = [self.pst[0], self.pst[1]]
        Oq = [self.psb[4], self.psb[5]]
        P.dma("sp", kr.ap[:, :], kr_s.ap[:, :], reads=[kr_s], writes=[kr])

        def load_head(h):
            P.dma("sp", qn[h % 2].ap[:, :], qn_s.ap[h], reads=[qn_s], writes=[qn[h % 2]])
            P.dma("sp", kn[h % 2].ap[:, :], kn_s.ap[h], reads=[kn_s], writes=[kn[h % 2]])
            P.dma("sp", qr[h % 2].ap[:, :], qr_s.ap[h // 2], reads=[qr_s], writes=[qr[h % 2]])
            P.dma("sp", vv[h % 2].ap[:, :, :], v_s.ap[:, h * 128:(h + 1) * 128].rearrange("(b p) n -> p b n", p=128),
                  reads=[v_s], writes=[vv[h % 2]])

        chunks = []
        qbi = 0
        for h in range(8):
            for i in range(NB):
                cl = []
                for j0 in range(0, i * 128, 512):
                    cl.append(dict(j0=j0, w=min(512, i * 128 - j0), diag=False))
                cl.append(dict(j0=i * 128, w=128, diag=True))
                for ci, c in enumerate(cl):
                    c.update(h=h, i=i, ci=ci, nc=len(cl), last=(ci == len(cl) - 1), qb=qbi, first=(ci == 0))
                    chunks.append(c)
                qbi += 1
        NCH = len(chunks)
        first_of_head = {}
        for n, c in enumerate(chunks):
            first_of_head.setdefault(c["h"], n)
        cnt = {"ev": 0, "tp": 0}
        pend = []

        def stA(n):
            c = chunks[n]
            h, i, j0, w = c["h"], c["i"], c["j0"], c["w"]
            q_, k_, r_ = qn[h % 2], kn[h % 2], qr[h % 2]
            hp = h % 2
            ps = psS[n % 4]
            self.mm(ps.ap[:, :w], q_.ap[:, i * 128:(i + 1) * 128], k_.ap[:, j0:j0 + w], True, False, [q_, k_], [ps])
            self.mm(ps.ap[:, :w], r_.ap[hp * 64:(hp + 1) * 64, i * 128:(i + 1) * 128], kr.ap[hp * 64:(hp + 1) * 64, j0:j0 + w],
                    False, True, [r_, kr], [ps])

        def stB(n):
            c = chunks[n]
            w, ci, q = c["w"], c["ci"], c["qb"] % NQ
            ps = psS[n % 4]
            m_, nb_, l_ = mst[q], nbr[n % 8], lst[q]
            if c["first"]:
                flush(c["qb"] - NQ)
                P.emit("dve", lambda E: E.memset(l_.ap[:, :], 0.0), writes=[l_])
            if c["diag"]:
                sd = sdg[c["qb"] % 2]
                self.tt("dve", sd.ap[:, :], ps.ap[:, 0:128], cmask.ap[:, :], ALU.add, [ps, cmask], [sd])
                src, srcb = sd.ap[:, :], sd
            else:
                src, srcb = ps.ap[:, :w], ps
            P.emit("dve", lambda E: E.tensor_reduce(m_.ap[:, ci:ci + 1], src, AX.X, ALU.max), reads=[srcb], writes=[m_])
            self.ts("dve", nb_.ap[:, 0:1], m_.ap[:, ci:ci + 1], -scale, None, ALU.mult, None, [m_], [nb_])
            p_ = pb[n % 4]
            self.act(p_.ap[:, :w], src, AF.Exp, [srcb, nb_], [p_, l_], bias=nb_.ap[:, 0:1], scale=scale, accum=l_.ap[:, ci:ci + 1])
            for _ in range(3):
                if pend:
                    pend.pop(0)[1]()

        def flush(upto_qb):
            while pend and pend[0][0] <= upto_qb:
                pend.pop(0)[1]()

        def stT(n):
            c = chunks[n]
            w = c["w"]
            p_ = pb[n % 4]
            tp_ = ptp[n % 2]
            for j in range(w // 128):
                P.emit("pe", lambda E, j=j, tp_=tp_, p_=p_: E.transpose(tp_.ap[:, j * 128:(j + 1) * 128], p_.ap[:, j * 128:(j + 1) * 128], ident_b.ap[:, :]),
                       reads=[p_, ident_b], writes=[tp_])

        def stD(n):
            c = chunks[n]
            nb = c["w"] // 128
            tp_ = ptp[n % 2]
            pt_ = pts[n % 4]
            self.copy("act", pt_.ap[:, 0:nb, :], tp_.ap[:, 0:nb * 128].rearrange("p (a b) -> p a b", a=nb), [tp_], [pt_])

        def stE(n):
            c = chunks[n]
            h, i, j0, w, ci, q = c["h"], c["i"], c["j0"], c["w"], c["ci"], c["qb"] % NQ
            v_ = vv[h % 2]
            pt_ = pts[n % 4]
            o_ = Oq[n % 2]
            nb = w // 128
            for j in range(nb):
                self.mm(o_.ap[:, 0:128], pt_.ap[:, j, :], v_.ap[:, j0 // 128 + j, :], j == 0, j == nb - 1, [pt_, v_], [o_])
            oc_ = ocs[q]
            self.copy("act", oc_.ap[:, ci, :], o_.ap[:, 0:128], [o_], [oc_])
            if c["last"]:
                combine(n)
            if n == first_of_head[h] and h >= 1 and h + 1 < 8:
                load_head(h + 1)

        def combine(n):
            c = chunks[n]
            h, i, nc, q, qb = c["h"], c["i"], c["nc"], c["qb"] % NQ, c["qb"]
            m_, l_, w_, c_, oc_ = mst[q], lst[q], wst[q], cst[q], ocs[q]
            oa = oacc[qb % 2]
            o_ = on[qb % 2]
            ost = otst[h % 2]
            mo = []
            mo.append(lambda: P.emit("dve", lambda E: E.tensor_reduce(c_.ap[:, 0:1], m_.ap[:, 0:nc], AX.X, ALU.max), reads=[m_], writes=[c_]))
            mo.append(lambda: self.ts("dve", w_.ap[:, 0:nc], m_.ap[:, 0:nc], c_.ap[:, 0:1], None, ALU.subtract, None, [m_, c_], [w_]))
            mo.append(lambda: self.act(w_.ap[:, 0:nc], w_.ap[:, 0:nc], AF.Exp, [w_], [w_], scale=scale))
            mo.append(lambda: self.tt("dve", l_.ap[:, 0:nc], l_.ap[:, 0:nc], w_.ap[:, 0:nc], ALU.mult, [l_, w_], [l_]))
            mo.append(lambda: P.emit("dve", lambda E: E.tensor_reduce(c_.ap[:, 1:2], l_.ap[:, 0:nc], AX.X, ALU.add), reads=[l_], writes=[c_]))
            mo.append(lambda: P.emit("dve", lambda E: E.reciprocal(c_.ap[:, 2:3], c_.ap[:, 1:2]), reads=[c_], writes=[c_]))
            mo.append(lambda: self.ts("dve", w_.ap[:, 0:nc], w_.ap[:, 0:nc], c_.ap[:, 2:3], None, ALU.mult, None, [w_, c_], [w_]))
            for k in range(nc):
                dst = o_ if k == nc - 1 else oa
                if k == 0:
                    mo.append(lambda dst=dst: self.ts("dve", dst.ap[:, :], oc_.ap[:, 0, :], w_.ap[:, 0:1], None, ALU.mult, None, [oc_, w_], [dst]))
                else:
                    mo.append(lambda dst=dst, k=k: self.stt("dve", dst.ap[:, :], oc_.ap[:, k, :], w_.ap[:, k:k + 1], oa.ap[:, :], ALU.mult, ALU.add, [oc_, w_, oa], [dst]))

            def tail():
                tp_ = ptp[cnt["tp"] % 2]
                cnt["tp"] += 1
                P.emit("pe", lambda E: E.transpose(tp_.ap[:, 0:128], o_.ap[:, :], ident_b.ap[:, :]), reads=[o_, ident_b], writes=[tp_])
                self.copy("act", ost.ap[:, i * 128:(i + 1) * 128], tp_.ap[:, 0:128], [tp_], [ost])
                if i == NB - 1:
                    P.dma("act", ot_s.ap[h * 128:(h + 1) * 128, :], ost.ap[:, :], reads=[ost], writes=[ot_s])
            mo.append(tail)
            for f in mo:
                pend.append((qb, f))

        load_head(0)
        load_head(1)
        LA = 4
        for s_ in range(-LA, NCH):
            if 0 <= s_ + LA < NCH:
                stA(s_ + LA)
                stB(s_ + LA)
            if 0 <= s_ + 2 < NCH:
                stT(s_ + 2)
                stD(s_ + 2)
            if 0 <= s_ < NCH:
                stE(s_)
        flush(10 ** 9)

    def attn_sb(self, qsb_s, ksb_s, vsb_s, ot2_s, triU, triL, m01):
        P, S, NB, NT = self.P, self.S, self.NB, self.NT
        scale = 1.0 / math.sqrt(128.0)
        qq = [[self.lalloc(f"qq{c}{i}", [128, S], BF16, dma=True) for i in range(2)] for c in range(2)]
        kk = [[self.lalloc(f"kk{c}{i}", [128, S], BF16, dma=True) for i in range(2)] for c in range(2)]
        vv = [[self.lalloc(f"vv{c}{i}", [128, NB, 128], BF16, dma=True) for i in range(2)] for c in range(2)]
        Eb = [[self.lalloc(f"E{c}{i}", [128, 512], F32) for i in range(4)] for c in range(2)]
        SPb = [[self.lalloc(f"SPb{c}{i}", [128, 512], BF16) for i in range(4)] for c in range(2)]
        Gb = [[self.lalloc(f"G{c}{i}", [128, 512], F32) for i in range(3)] for c in range(2)]
        Ab = [[self.lalloc(f"A{c}{i}", [128, 512], BF16) for i in range(4)] for c in range(2)]
        otst = [self.lalloc(f"otst{c}", [128, S], BF16, dma=True) for c in range(2)]
        Xp = [self.psb[4], self.psb[5]]
        Op = [self.psb[6], self.psb[7]]

        def load_pair(pr):
            for c in range(2):
                h = 2 * pr + c
                P.dma("sp", qq[c][pr % 2].ap[:, :], qsb_s.ap[h], reads=[qsb_s], writes=[qq[c][pr % 2]])
                P.dma("sp", kk[c][pr % 2].ap[:, :], ksb_s.ap[h], reads=[ksb_s], writes=[kk[c][pr % 2]])
                P.dma("sp", vv[c][pr % 2].ap[:, :, :], vsb_s.ap[:, h * 128:(h + 1) * 128].rearrange("(b p) n -> p b n", p=128),
                      reads=[vsb_s], writes=[vv[c][pr % 2]])

        it = 0
        load_pair(0)
        for pr in range(4):
            if pr + 1 < 4:
                load_pair(pr + 1)
            for j in range(NT):
                q0 = j * 512
                kbs = list(range(4 * j + 3, -1, -1))
                nk = len(kbs)
                st = [dict(), dict()]

                zps = [dict(), dict()]

                def zmm(c, n):
                    q_, k_ = qq[c][pr % 2], kk[c][pr % 2]
                    kb = kbs[n]
                    ps = self.next_ps()
                    self.mm(ps.ap[:, :], k_.ap[:, kb * 128:(kb + 1) * 128], q_.ap[:, q0:q0 + 512], True, True, [k_, q_], [ps])
                    zps[c][n] = ps

                def zstage(c, n):
                    kb = kbs[n]
                    o = kb - 4 * j
                    ps = zps[c].pop(n)
                    e_ = Eb[c][(it + n) % 4]
                    self.act(e_.ap[:, :], ps.ap[:, :], AF.Exp, [ps], [e_], scale=scale)
                    if o >= 0:
                        self.tt("dve", e_.ap[:, :], e_.ap[:, :], m01[o].ap[:, :], ALU.mult, [e_, m01[o]], [e_])
                    st[c][n] = (e_,)

                def zstage2(c, n):
                    (e_,) = st[c][n]
                    sb_ = SPb[c][(it + n) % 4]
                    self.act(sb_.ap[:, :], e_.ap[:, :], AF.Ln, [e_], [sb_], bias=1.0)
                    st[c][n] = (e_, sb_)

                def t1(c, n):
                    e_, sb_ = st[c][n]
                    P.emit("pe", lambda E, c=c, n=n, sb_=sb_: E.matmul(Xp[c].ap[:, :], triU.ap[:, :], sb_.ap[:, :], start=(n == 0), stop=True, skip_group_check=True),
                           reads=[triU, sb_], writes=[Xp[c]])

                def t2(c, n):
                    g_ = Gb[c][(it + n) % 3]
                    self.act(g_.ap[:, :], Xp[c].ap[:, :], AF.Exp, [Xp[c]], [g_], scale=-1.0)
                    st[c][n] = st[c][n] + (g_,)

                def t3(c, n):
                    e_, sb_, g_ = st[c][n]
                    if n < nk - 1:
                        P.emit("pe", lambda E, c=c, sb_=sb_: E.matmul(Xp[c].ap[:, :], triL.ap[:, :], sb_.ap[:, :], start=False, stop=True, skip_group_check=True),
                               reads=[triL, sb_], writes=[Xp[c]])

                def t4(c, n):
                    e_, sb_, g_ = st[c][n]
                    a_ = Ab[c][(it + n) % 4]
                    self.tt("dve", a_.ap[:, :], e_.ap[:, :], g_.ap[:, :], ALU.mult, [e_, g_], [a_])
                    st[c][n] = (a_,)

                def pstage(c, n):
                    (a_,) = st[c][n]
                    kb = kbs[n]
                    v_ = vv[c][pr % 2]
                    self.mm(Op[c].ap[:, :], v_.ap[:, kb, :], a_.ap[:, :], n == 0, n == nk - 1, [v_, a_], [Op[c]])

                for c in range(2):
                    zmm(c, 0)
                if nk > 1:
                    for c in range(2):
                        zmm(c, 1)
                for c in range(2):
                    zstage(c, 0)
                for c in range(2):
                    zstage2(c, 0)
                for n in range(nk):
                    if n + 1 < nk:
                        for c in range(2):
                            zstage(c, n + 1)
                        for c in range(2):
                            zstage2(c, n + 1)
                    for c in range(2):
                        t1(c, n)
                    if n + 2 < nk:
                        for c in range(2):
                            zmm(c, n + 2)
                    for c in range(2):
                        t2(c, n)
                    for c in range(2):
                        t3(c, n)
                    for c in range(2):
                        t4(c, n)
                    if n >= 1:
                        for c in range(2):
                            pstage(c, n - 1)
                for c in range(2):
                    pstage(c, nk - 1)
                it += nk
                for c in range(2):
                    self.copy("act", otst[c].ap[:, q0:q0 + 512], Op[c].ap[:, :], [Op[c]], [otst[c]])
            for c in range(2):
                h = 2 * pr + c
                P.dma("act", ot2_s.ap[h * 128:(h + 1) * 128, :], otst[c].ap[:, :], reads=[otst[c]], writes=[ot2_s])


_NC_CACHE = {}


def build_nc(S):
    if S in _NC_CACHE:
        return _NC_CACHE[S]
    nc = bass.Bass("TRN2", target_bir_lowering=False)
    with ExitStack() as es:
        kb = KB(nc, es, S)
        stats = kb.build()
    _NC_CACHE[S] = nc
    return nc


def _pc(v, nch):
    return np.ascontiguousarray(np.asarray(v, np.float32).reshape(nch, 128).T)


def prep_shared(w_mod, b_mod, g_mix, g_ffn, w_a_down, g_q_lat, g_kv_lat, w_uq, w_ukv, w_oa, w_mod_kv, b_mod_kv,
                g_kv, w_kv_sb, w_q_sb, w_o_sb, w_ffn_gu, w_ffn_down, w_router, b_router, w_exp_gu, w_exp_down, g_final):
    f = lambda a: np.ascontiguousarray(np.asarray(a, np.float32))
    sh = {}
    sh["w_mod"] = f(w_mod)
    sh["w_mod_kv"] = f(w_mod_kv)
    sh["bmod"] = np.concatenate([_pc(b_mod[0], 48), _pc(b_mod[1], 48), _pc(b_mod_kv, 16)], axis=1)
    sh["gvec"] = np.concatenate([_pc(g_mix[0], 8), _pc(g_mix[1], 8), _pc(g_ffn[0], 8), _pc(g_ffn[1], 8),
                                 _pc(g_kv, 8), _pc(g_final, 8)], axis=1)
    sh["glat"] = np.concatenate([_pc(g_q_lat[0], 3), _pc(g_kv_lat[0], 2)], axis=1)
    sh["brout"] = f(b_router[0]).reshape(1, NE)
    sh["wrout"] = f(w_router[0])
    p = np.arange(128)
    inv = (10000.0 ** (-(p % 32).astype(np.float64) / 32.0)).astype(np.float32)
    sgn = np.where((p % 64) < 32, -1.0, 1.0).astype(np.float32)
    sh["ropec"] = np.ascontiguousarray(np.stack([inv, sgn], axis=1))
    wad = f(w_a_down[0])
    kr1 = np.arange(640, 672)
    kr2 = np.arange(672, 704)
    colsA = np.concatenate([kr1, kr2, kr1, kr2])
    colsB = np.concatenate([kr2, kr1, kr2, kr1])
    sh["wad"] = np.ascontiguousarray(np.concatenate([wad[:, :640], wad[:, colsA], wad[:, colsB]], axis=1))
    wuq = f(w_uq[0])
    cn = np.concatenate([np.arange(h * 192, h * 192 + 128) for h in range(8)])
    ca, cb = [], []
    for j in range(4):
        for h in (2 * j, 2 * j + 1):
            x1 = np.arange(h * 192 + 128, h * 192 + 160)
            x2 = np.arange(h * 192 + 160, h * 192 + 192)
            ca += [x1, x2]
            cb += [x2, x1]
    sh["wuq"] = np.ascontiguousarray(np.concatenate([wuq[:, cn], wuq[:, np.concatenate(ca)], wuq[:, np.concatenate(cb)]], axis=1))
    wukv = f(w_ukv[0])
    ck = np.concatenate([np.arange(h * 256, h * 256 + 128) for h in range(8)])
    cv = np.concatenate([np.arange(h * 256 + 128, h * 256 + 256) for h in range(8)])
    sh["wukv"] = np.ascontiguousarray(np.concatenate([wukv[:, ck], wukv[:, cv]], axis=1))
    sh["woa"] = f(w_oa[0])
    gu_cols = np.concatenate([np.concatenate([np.arange(c2 * 128, c2 * 128 + 256), DFF + np.arange(c2 * 128, c2 * 128 + 256)])
                              for c2 in range(0, 28, 2)])
    sh["wgu"] = np.ascontiguousarray(f(w_ffn_gu[0])[:, gu_cols])
    sh["wdn"] = f(w_ffn_down[0])
    sh["wkvsb"] = f(w_kv_sb)
    sh["wqsb"] = f(w_q_sb[0])
    sh["wosb"] = f(w_o_sb[0])
    sh["wegu"] = np.ascontiguousarray(f(w_exp_gu[0])[:, :, gu_cols]).reshape(NE * D, 2 * DFF)
    sh["wedn"] = f(w_exp_down[0]).reshape(NE * DFF, D)
    return sh


def run(x, c, positions, sh, S, ncores):
    nc = build_nc(S)
    in_maps = []
    for b in range(ncores):
        m = dict(sh)
        m["xT"] = np.ascontiguousarray(np.asarray(x[b], np.float32).T)
        m["cvec"] = _pc(c[b], 8)
        m["pos"] = np.ascontiguousarray(np.asarray(positions[b], np.int32).reshape(1, S))
        in_maps.append(m)
    res = run_bass_kernel_spmd(nc, in_maps, core_ids=list(range(ncores)))
    out = np.stack([np.ascontiguousarray(res.results[b]["outT"].T) for b in range(ncores)], axis=0)
    return out.astype(np.float32)


def kernel(x, c, positions, w_mod, b_mod, g_mix, g_ffn, w_a_down, g_q_lat, g_kv_lat, w_uq, w_ukv, w_oa,
           w_mod_kv, b_mod_kv, g_kv, w_kv_sb, w_q_sb, w_o_sb, w_ffn_gu, w_ffn_down, w_router, b_router,
           w_exp_gu, w_exp_down, g_final):
    sh = prep_shared(w_mod, b_mod, g_mix, g_ffn, w_a_down, g_q_lat, g_kv_lat, w_uq, w_ukv, w_oa, w_mod_kv, b_mod_kv,
                     g_kv, w_kv_sb, w_q_sb, w_o_sb, w_ffn_gu, w_ffn_down, w_router, b_router, w_exp_gu, w_exp_down, g_final)
    x = np.asarray(x)
    return run(x, np.asarray(c), np.asarray(positions), sh, x.shape[1], x.shape[0])
```
